# Optimizing a Trainium2 kernel written in Bass

```python
import math
import jax
import jax.numpy as jnp
from jax import lax
import numpy as np

D_MODEL = 4096
BATCH = 1
SEQ = 8192
DEPTH = 2

CTX_LEN = 256
GRID_W = 64
N_BRANCH = 4
HEAD_DIM = 128
BRANCH_DIM = 1024
EPS = 1e-6
ROPE_THETA = 10000.0

NA_HEADS = 8
NA_WIN_R = 8
NA_WIN_C = 16
NA_DIM = NA_HEADS * HEAD_DIM

GLA_HEADS = 8
GLA_DK = 64
GLA_DV = 128
GLA_RANK = 16
GLA_TAU = 16.0
GLA_CHUNK = 64
GLA_QK_DIM = GLA_HEADS * GLA_DK
GLA_V_DIM = GLA_HEADS * GLA_DV

MLA_HEADS = 8
MLA_Q_RANK = 1024
MLA_KV_RANK = 512
MLA_NOPE = 128
MLA_ROPE = 64
MLA_V = 128
MLA_QBLOCK = 128
MLA_SCALE = (MLA_NOPE + MLA_ROPE) ** -0.5

GDN_HEADS = 8
GDN_DK = 128
GDN_DV = 128
GDN_CONV = 5
GDN_CHUNK = 64
GDN_QK_DIM = GDN_HEADS * GDN_DK
GDN_V_DIM = GDN_HEADS * GDN_DV

N_EXPERTS = 16
EC_CAPACITY_FACTOR = 2
D_EXPERT = 1024

IN_LAYOUT = (
    ('na_q', NA_DIM), ('na_k', NA_DIM), ('na_v', NA_DIM),
    ('gla_q', GLA_QK_DIM), ('gla_k', GLA_QK_DIM), ('gla_v', GLA_V_DIM), ('gla_g', GLA_V_DIM),
    ('gla_lr', 2 * GLA_RANK),
    ('mla_cq', MLA_Q_RANK), ('mla_ckv', MLA_KV_RANK), ('mla_kr', MLA_ROPE),
    ('gdn_qkv', 2 * GDN_QK_DIM + GDN_V_DIM), ('gdn_z', GDN_V_DIM),
    ('gdn_b', 2 * GDN_HEADS), ('gdn_a', 2 * GDN_HEADS),
    ('gates', N_BRANCH * D_MODEL),
)
IN_WIDTH = sum(w for _, w in IN_LAYOUT)

kernel_name = 'hybrid_dit_parallel_mixers_ec_moe'


def rmsnorm(x, w):
    xf = x.astype(jnp.float32)
    y = xf * lax.rsqrt(jnp.mean(xf * xf, axis=-1, keepdims=True) + EPS)
    return (y * w.astype(jnp.float32)).astype(x.dtype)


def l2norm(x):
    return x * lax.rsqrt(jnp.sum(x * x, axis=-1, keepdims=True) + EPS)


def heads(t, n, d):
    return t.reshape(t.shape[:-1] + (n, d))


def split_proj(p):
    out, off = {}, 0
    for name, w in IN_LAYOUT:
        out[name] = p[..., off:off + w]
        off += w
    return out


def axial_rope(x, row, col):
    half = x.shape[-1] // 2
    nf = half // 2
    inv = ROPE_THETA ** (-jnp.arange(nf, dtype=jnp.float32) / nf)
    xf = x.astype(jnp.float32)

    def rot(xa, p):
        ang = p.astype(jnp.float32)[:, None] * inv[None, :]
        cos, sin = jnp.cos(ang)[:, None, :], jnp.sin(ang)[:, None, :]
        x1, x2 = xa[..., :nf], xa[..., nf:]
        return jnp.concatenate([x1 * cos - x2 * sin, x1 * sin + x2 * cos], axis=-1)

    return jnp.concatenate([rot(xf[..., :half], row), rot(xf[..., half:], col)], axis=-1).astype(x.dtype)


def softmax_attention(q, k, v, scale):
    s = jnp.einsum('bqhd,bkhd->bhqk', q, k).astype(jnp.float32) * scale
    p = jax.nn.softmax(s, axis=-1).astype(v.dtype)
    return jnp.einsum('bhqk,bkhd->bqhd', p, v)


def blocked_attention(q, k, v, scale):
    B, Nq, H, dk = q.shape
    nb = Nq // MLA_QBLOCK
    qb = jnp.moveaxis(q.reshape(B, nb, MLA_QBLOCK, H, dk), 1, 0)
    o = lax.map(lambda qblk: softmax_attention(qblk, k, v, scale), qb)
    return jnp.moveaxis(o, 0, 1).reshape(B, Nq, H, v.shape[-1])


def neighborhood_attention(q, k, v, k_ctx, v_ctx, rpb):
    B, N, H, Dh = q.shape
    rows = N // GRID_W
    wr = min(NA_WIN_R, rows)
    scale = Dh ** -0.5
    grid = lambda t: t.reshape(B, rows, GRID_W, H, Dh)
    qg, kg, vg = grid(q), grid(k), grid(v)
    r = jnp.arange(rows)
    r0 = jnp.clip(r - wr // 2, 0, rows - wr)
    row_idx = r0[:, None] + jnp.arange(wr)[None, :]
    col = jnp.arange(GRID_W)
    c0 = jnp.clip(col - NA_WIN_C // 2, 0, GRID_W - NA_WIN_C)
    col_ok = (col[None, :] >= c0[:, None]) & (col[None, :] < c0[:, None] + NA_WIN_C)
    kr = kg[:, row_idx]
    vr = vg[:, row_idx]
    dr = row_idx - r[:, None] + NA_WIN_R - 1
    dc = jnp.clip(col[None, :] - col[:, None] + NA_WIN_C - 1, 0, 2 * NA_WIN_C - 2)
    bias = rpb[:, dr[:, None, :, None], dc[None, :, None, :]].transpose(1, 2, 0, 3, 4).astype(jnp.float32)
    s_lat = jnp.einsum('brqhd,brikhd->brqhik', qg, kr).astype(jnp.float32) * scale + bias[None]
    s_lat = jnp.where(col_ok[None, None, :, None, None, :], s_lat, -jnp.inf)
    s_ctx = jnp.einsum('brqhd,bchd->brqhc', qg, k_ctx).astype(jnp.float32) * scale
    n_lat = wr * GRID_W
    s = jnp.concatenate([s_lat.reshape(B, rows, GRID_W, H, n_lat), s_ctx], axis=-1)
    p = jax.nn.softmax(s, axis=-1).astype(v.dtype)
    o = (jnp.einsum('brqhik,brikhd->brqhd', p[..., :n_lat].reshape(B, rows, GRID_W, H, wr, GRID_W), vr)
         + jnp.einsum('brqhc,bchd->brqhd', p[..., n_lat:], v_ctx))
    return o.reshape(B, N, H, Dh)


def gla_states(xs, s0):
    q, k, v, la = xs
    B, T, H, dk = k.shape
    n = T // GLA_CHUNK
    kc = k.reshape(B, n, GLA_CHUNK, H, dk)
    vc = v.reshape(B, n, GLA_CHUNK, H, -1)
    bc = jnp.cumsum(la.reshape(B, n, GLA_CHUNK, H, dk), axis=2)
    b_last = bc[:, :, -1]
    ds = jnp.einsum('bnchk,bnchv->nbhkv', kc * jnp.exp(b_last[:, :, None] - bc), vc)
    dec = jnp.moveaxis(jnp.exp(b_last), 1, 0)

    def step(S, inp):
        d, dS = inp
        return S * d[..., None] + dS, S

    s_fin, s_prev = lax.scan(step, s0, (dec, ds))
    return (s_prev, bc), s_fin


def gla_outputs(xs, aux):
    q, k, v, la = xs
    s_prev, bc = aux
    B, T, H, dk = q.shape
    n = T // GLA_CHUNK
    qc = q.reshape(B, n, GLA_CHUNK, H, dk)
    kc = k.reshape(B, n, GLA_CHUNK, H, dk)
    vc = v.reshape(B, n, GLA_CHUNK, H, -1)
    mid = bc[:, :, GLA_CHUNK // 2:GLA_CHUNK // 2 + 1]
    s = jnp.einsum('bnihk,bnjhk->bnhij', qc * jnp.exp(bc - mid), kc * jnp.exp(mid - bc))
    s = jnp.where(jnp.tril(jnp.ones((GLA_CHUNK, GLA_CHUNK), bool)), s, 0.0)
    o = (jnp.einsum('bnhij,bnjhv->bnihv', s, vc)
         + jnp.einsum('bnihk,nbhkv->bnihv', qc * jnp.exp(bc), s_prev))
    return o.reshape(B, T, H, -1)


def gdn_states(xs, s0):
    q, k, v, g, beta = xs
    B, T, H, dk = k.shape
    C = GDN_CHUNK
    n = T // C
    chunks = lambda t: jnp.swapaxes(t.reshape((B, n, C, H) + t.shape[3:]), 2, 3)
    kc, vc, bc = chunks(k), chunks(v), chunks(beta)
    gc = jnp.cumsum(chunks(g), axis=-1)
    tri = jnp.tril(jnp.ones((C, C), bool))
    decay = jnp.exp(jnp.where(tri, gc[..., :, None] - gc[..., None, :], -jnp.inf))
    kb = kc * bc[..., None]
    strict = jnp.tril(jnp.ones((C, C), bool), -1)
    eye = jnp.eye(C, dtype=kc.dtype)
    lower = jnp.where(strict, jnp.einsum('bnhid,bnhjd->bnhij', kb, kc) * decay, 0.0) + eye
    tmat = lax.linalg.triangular_solve(lower, jnp.broadcast_to(eye, lower.shape),
                                       left_side=True, lower=True, unit_diagonal=True)
    u = tmat @ (vc * bc[..., None])
    w = tmat @ (kb * jnp.exp(gc)[..., None])
    g_last = gc[..., -1]
    kd = kc * jnp.exp(g_last[..., None] - gc)[..., None]
    mv = lambda t: jnp.moveaxis(t, 1, 0)

    def step(S, inp):
        u_c, w_c, kd_c, gl_c = inp
        v_new = u_c - jnp.einsum('bhck,bhkv->bhcv', w_c, S)
        s_next = S * jnp.exp(gl_c)[..., None, None] + jnp.einsum('bhck,bhcv->bhkv', kd_c, v_new)
        return s_next, (S, v_new)

    s_fin, (s_prev, v_new) = lax.scan(step, s0, (mv(u), mv(w), mv(kd), mv(g_last)))
    return (s_prev, v_new, gc, decay), s_fin


def gdn_outputs(xs, aux):
    q, k = xs[0], xs[1]
    s_prev, v_new, gc, decay = aux
    B, T, H, dk = q.shape
    n = T // GDN_CHUNK
    chunks = lambda t: jnp.swapaxes(t.reshape(B, n, GDN_CHUNK, H, dk), 2, 3)
    qc, kc = chunks(q), chunks(k)
    attn = jnp.einsum('bnhid,bnhjd->bnhij', qc, kc) * decay
    o = (jnp.einsum('bnhij,nbhjv->bnhiv', attn, v_new)
         + jnp.einsum('bnhid,nbhdv->bnhiv', qc * jnp.exp(gc)[..., None], s_prev))
    return jnp.swapaxes(o, 2, 3).reshape(B, T, H, -1)


def run_direction(states_fn, outputs_fn, lat, ctx, s0, ctx_out):
    aux_c, s_c = states_fn(ctx, s0)
    aux_l, _ = states_fn(lat, s_c)
    o_lat = outputs_fn(lat, aux_l)
    o_ctx = outputs_fn(ctx, aux_c) if ctx_out else None
    return o_lat, o_ctx


def bidirectional(states_fn, outputs_fn, lat_f, lat_b, ctx_f, ctx_b, s0, ctx_out):
    flip = lambda xs: tuple(jnp.flip(t, axis=1) for t in xs)
    of, ofc = run_direction(states_fn, outputs_fn, lat_f, ctx_f, s0, ctx_out)
    ob, obc = run_direction(states_fn, outputs_fn, flip(lat_b), flip(ctx_b), s0, ctx_out)
    o = of + jnp.flip(ob, axis=1)
    oc = (ofc + jnp.flip(obc, axis=1)) if ctx_out else None
    return o, oc


def centred_conv(x, w):
    K, ch = w.shape
    return lax.conv_general_dilated(x, w[:, None, :].astype(x.dtype), window_strides=(1,),
                                    padding=[(K // 2, K // 2)],
                                    dimension_numbers=('NWC', 'WIO', 'NWC'),
                                    feature_group_count=ch)


def gla_mixer(P, Pc, w2, b2, norm_w, ctx_out):
    def prep(p):
        q = heads(p['gla_q'], GLA_HEADS, GLA_DK).astype(jnp.float32) * GLA_DK ** -0.5
        k = heads(p['gla_k'], GLA_HEADS, GLA_DK).astype(jnp.float32)
        v = heads(p['gla_v'], GLA_HEADS, GLA_DV).astype(jnp.float32)
        out = []
        for d in range(2):
            z = p['gla_lr'][..., d * GLA_RANK:(d + 1) * GLA_RANK] @ w2[d] + b2[d]
            la = jax.nn.log_sigmoid(z.astype(jnp.float32)) / GLA_TAU
            out.append((q, k, v, heads(la, GLA_HEADS, GLA_DK)))
        return out

    lat_f, lat_b = prep(P)
    ctx_f, ctx_b = prep(Pc)
    s0 = jnp.zeros((P['gla_q'].shape[0], GLA_HEADS, GLA_DK, GLA_DV), jnp.float32)
    o, oc = bidirectional(gla_states, gla_outputs, lat_f, lat_b, ctx_f, ctx_b, s0, ctx_out)

    def finish(o, g):
        y = rmsnorm(o, norm_w.reshape(GLA_HEADS, GLA_DV)) * jax.nn.silu(heads(g, GLA_HEADS, GLA_DV).astype(jnp.float32))
        return y.reshape(g.shape).astype(g.dtype)

    return finish(o, P['gla_g']), (finish(oc, Pc['gla_g']) if ctx_out else None)


def gdn_mixer(P, Pc, conv_w, a_log, dt_bias, norm_w, ctx_out):
    def prep(p):
        y = jax.nn.silu(centred_conv(p['gdn_qkv'], conv_w)).astype(jnp.float32)
        q = l2norm(heads(y[..., :GDN_QK_DIM], GDN_HEADS, GDN_DK)) * GDN_DK ** -0.5
        k = l2norm(heads(y[..., GDN_QK_DIM:2 * GDN_QK_DIM], GDN_HEADS, GDN_DK))
        v = heads(y[..., 2 * GDN_QK_DIM:], GDN_HEADS, GDN_DV)
        out = []
        for d in range(2):
            sl = slice(d * GDN_HEADS, (d + 1) * GDN_HEADS)
            beta = jax.nn.sigmoid(p['gdn_b'][..., sl].astype(jnp.float32))
            g = -jnp.exp(a_log[d].astype(jnp.float32)) * jax.nn.softplus(
                p['gdn_a'][..., sl].astype(jnp.float32) + dt_bias[d].astype(jnp.float32))
            out.append((q, k, v, g, beta))
        return out

    lat_f, lat_b = prep(P)
    ctx_f, ctx_b = prep(Pc)
    s0 = jnp.zeros((P['gdn_qkv'].shape[0], GDN_HEADS, GDN_DK, GDN_DV), jnp.float32)
    o, oc = bidirectional(gdn_states, gdn_outputs, lat_f, lat_b, ctx_f, ctx_b, s0, ctx_out)

    def finish(o, z):
        y = rmsnorm(o, norm_w) * jax.nn.silu(heads(z, GDN_HEADS, GDN_DV).astype(jnp.float32))
        return y.reshape(z.shape).astype(z.dtype)

    return finish(o, P['gdn_z']), (finish(oc, Pc['gdn_z']) if ctx_out else None)


def mla_q(p, norm_w, w_uq, pos):
    q = heads(rmsnorm(p['mla_cq'], norm_w) @ w_uq, MLA_HEADS, MLA_NOPE + MLA_ROPE)
    if pos is None:
        return q
    return jnp.concatenate([q[..., :MLA_NOPE], axial_rope(q[..., MLA_NOPE:], *pos)], axis=-1)


def mla_kv(p, norm_w, w_ukv, pos):
    kv = heads(rmsnorm(p['mla_ckv'], norm_w) @ w_ukv, MLA_HEADS, MLA_NOPE + MLA_V)
    k_rope = p['mla_kr'][:, :, None, :]
    if pos is not None:
        k_rope = axial_rope(k_rope, *pos)
    k_rope = jnp.broadcast_to(k_rope, kv.shape[:3] + (MLA_ROPE,))
    return jnp.concatenate([kv[..., :MLA_NOPE], k_rope], axis=-1), kv[..., MLA_NOPE:]


def gated_merge(branches, gates, w_br, w_o):
    D = w_o.shape[-1]
    y = None
    for i, o in enumerate(branches):
        term = jax.nn.sigmoid(gates[..., i * D:(i + 1) * D]) * (o @ w_br[i])
        y = term if y is None else y + term
    return y @ w_o


def token_mixers(u, uc, pos, w_in, na_rpb, gla_w2, gla_b2, gla_norm_w, mla_q_norm_w, mla_kv_norm_w,
                 mla_w_uq, mla_w_ukv, gdn_conv_w, gdn_a_log, gdn_dt_bias, gdn_norm_w, w_br, w_o, ctx_out):
    P = split_proj(u @ w_in)
    Pc = split_proj(uc @ w_in)
    flat = lambda t: t.reshape(t.shape[:2] + (-1,))
    na = lambda p, name: heads(p[name], NA_HEADS, HEAD_DIM)
    k_na_c, v_na_c = na(Pc, 'na_k'), na(Pc, 'na_v')
    o_a = flat(neighborhood_attention(na(P, 'na_q'), na(P, 'na_k'), na(P, 'na_v'), k_na_c, v_na_c, na_rpb))
    o_b, o_b_c = gla_mixer(P, Pc, gla_w2, gla_b2, gla_norm_w, ctx_out)
    k_c_c, v_c_c = mla_kv(Pc, mla_kv_norm_w, mla_w_ukv, None)
    k_c, v_c = mla_kv(P, mla_kv_norm_w, mla_w_ukv, pos)
    q_c = mla_q(P, mla_q_norm_w, mla_w_uq, pos)
    o_c = flat(blocked_attention(q_c, jnp.concatenate([k_c_c, k_c], axis=1),
                                 jnp.concatenate([v_c_c, v_c], axis=1), MLA_SCALE))
    o_d, o_d_c = gdn_mixer(P, Pc, gdn_conv_w, gdn_a_log, gdn_dt_bias, gdn_norm_w, ctx_out)
    y = gated_merge((o_a, o_b, o_c, o_d), P['gates'], w_br, w_o)
    if not ctx_out:
        return y, None
    o_a_c = flat(softmax_attention(na(Pc, 'na_q'), k_na_c, v_na_c, HEAD_DIM ** -0.5))
    o_c_c = flat(softmax_attention(mla_q(Pc, mla_q_norm_w, mla_w_uq, None), k_c_c, v_c_c, MLA_SCALE))
    yc = gated_merge((o_a_c, o_b_c, o_c_c, o_d_c), Pc['gates'], w_br, w_o)
    return y, yc


def expert_choice_ffn(x, router_w, w1, w3, w2):
    B, T, D = x.shape
    cap = EC_CAPACITY_FACTOR * T // N_EXPERTS
    aff = jax.nn.softmax((x @ router_w).astype(jnp.float32), axis=-1)
    gate, idx = lax.top_k(jnp.swapaxes(aff, 1, 2), cap)
    xs = jax.vmap(lambda xb, ib: xb[ib])(x, idx)
    hdn = jax.nn.silu(jnp.einsum('becd,edf->becf', xs, w1)) * jnp.einsum('becd,edf->becf', xs, w3)
    y = jnp.einsum('becf,efd->becd', hdn, w2) * gate[..., None].astype(x.dtype)
    scatter = lambda ib, yb: jnp.zeros((T, D), y.dtype).at[ib.reshape(-1)].add(yb.reshape(-1, D))
    return jax.vmap(scatter)(idx, y)


def setup_inputs(seed: int = 0) -> dict:
    key = jax.random.key(seed)
    ks = jax.random.split(key, 40)
    counter = [0]

    def nk():
        counter[0] += 1
        return ks[counter[0] - 1]

    def nrm(shape, scale):
        return jax.random.normal(nk(), shape, jnp.float32) * scale

    def gain(shape):
        return 1.0 + 0.02 * jax.random.normal(nk(), shape, jnp.float32)

    D, L = D_MODEL, DEPTH
    dt = jnp.exp(jax.random.uniform(nk(), (L, 2, GDN_HEADS), jnp.float32, math.log(1e-3), math.log(1e-1)))
    a_log = jnp.log(jax.random.uniform(nk(), (L, 2, GDN_HEADS), jnp.float32, 1.0, 16.0))
    return {
        'x': nrm((BATCH, SEQ, D), 1.0),
        'c': nrm((BATCH, D), 1.0),
        'ctx': nrm((BATCH, CTX_LEN, D), 1.0),
        'c_ctx': nrm((D,), 1.0),
        'ada_w': nrm((L, D, 6 * D), 0.5 * D ** -0.5),
        'ada_b': nrm((L, 6 * D), 0.02),
        'norm1_w': gain((L, D)),
        'w_in': nrm((L, D, IN_WIDTH), D ** -0.5),
        'na_rpb': nrm((L, NA_HEADS, 2 * NA_WIN_R - 1, 2 * NA_WIN_C - 1), 0.1),
        'gla_w2': nrm((L, 2, GLA_RANK, GLA_QK_DIM), GLA_RANK ** -0.5),
        'gla_b2': nrm((L, 2, GLA_QK_DIM), 0.1),
        'gla_norm_w': gain((L, GLA_V_DIM)),
        'mla_q_norm_w': gain((L, MLA_Q_RANK)),
        'mla_kv_norm_w': gain((L, MLA_KV_RANK)),
        'mla_w_uq': nrm((L, MLA_Q_RANK, MLA_HEADS * (MLA_NOPE + MLA_ROPE)), MLA_Q_RANK ** -0.5),
        'mla_w_ukv': nrm((L, MLA_KV_RANK, MLA_HEADS * (MLA_NOPE + MLA_V)), MLA_KV_RANK ** -0.5),
        'gdn_conv_w': nrm((L, GDN_CONV, 2 * GDN_QK_DIM + GDN_V_DIM), GDN_CONV ** -0.5),
        'gdn_a_log': a_log,
        'gdn_dt_bias': dt + jnp.log(-jnp.expm1(-dt)),
        'gdn_norm_w': gain((L, GDN_DV)),
        'w_br': nrm((L, N_BRANCH, BRANCH_DIM, D), BRANCH_DIM ** -0.5),
        'w_o': nrm((L, D, D), D ** -0.5),
        'norm2_w': gain((L, D)),
        'router_w': nrm((L, D, N_EXPERTS), D ** -0.5),
        'exp_w1': nrm((L, N_EXPERTS, D, D_EXPERT), D ** -0.5),
        'exp_w3': nrm((L, N_EXPERTS, D, D_EXPERT), D ** -0.5),
        'exp_w2': nrm((L, N_EXPERTS, D_EXPERT, D), D_EXPERT ** -0.5),
        'final_norm_w': gain((D,)),
    }


def reference(x, c, ctx, c_ctx, ada_w, ada_b, norm1_w, w_in, na_rpb, gla_w2, gla_b2, gla_norm_w,
              mla_q_norm_w, mla_kv_norm_w, mla_w_uq, mla_w_ukv, gdn_conv_w, gdn_a_log, gdn_dt_bias,
              gdn_norm_w, w_br, w_o, norm2_w, router_w, exp_w1, exp_w3, exp_w2, final_norm_w):
    N = x.shape[1]
    t = jnp.arange(N)
    pos = (t // GRID_W, t % GRID_W)
    h, hc = x, ctx
    for l in range(DEPTH):
        ctx_out = l < DEPTH - 1
        mod = jax.nn.silu(c) @ ada_w[l] + ada_b[l]
        mod_c = jax.nn.silu(c_ctx) @ ada_w[l] + ada_b[l]
        sh1, sc1, g1, sh2, sc2, g2 = jnp.split(mod[:, None, :], 6, axis=-1)
        csh1, csc1, cg1, csh2, csc2, cg2 = jnp.split(mod_c, 6)
        u = rmsnorm(h, norm1_w[l]) * (1.0 + sc1) + sh1
        uc = rmsnorm(hc, norm1_w[l]) * (1.0 + csc1) + csh1
        y, yc = token_mixers(u, uc, pos, w_in[l], na_rpb[l], gla_w2[l], gla_b2[l], gla_norm_w[l],
                             mla_q_norm_w[l], mla_kv_norm_w[l], mla_w_uq[l], mla_w_ukv[l],
                             gdn_conv_w[l], gdn_a_log[l], gdn_dt_bias[l], gdn_norm_w[l],
                             w_br[l], w_o[l], ctx_out)
        h = h + g1 * y
        u2 = rmsnorm(h, norm2_w[l]) * (1.0 + sc2) + sh2
        h = h + g2 * expert_choice_ffn(u2, router_w[l], exp_w1[l], exp_w3[l], exp_w2[l])
        if ctx_out:
            hc = hc + cg1 * yc
            uc2 = rmsnorm(hc, norm2_w[l]) * (1.0 + csc2) + csh2
            hc = hc + cg2 * expert_choice_ffn(uc2, router_w[l], exp_w1[l], exp_w3[l], exp_w2[l])
    return rmsnorm(h, final_norm_w)
```

```python
import numpy as np
from contextlib import ExitStack
import concourse.bass as bass
import concourse.mybir as mybir
from concourse.bass_utils import run_bass_kernel_spmd

F32 = mybir.dt.float32
BF16 = mybir.dt.bfloat16
I32 = mybir.dt.int32
U32 = mybir.dt.uint32
AF = mybir.ActivationFunctionType
ALU = mybir.AluOpType
AX = mybir.AxisListType
NDMASEM = 16


class T:
    def __init__(self, t, name):
        self.t = t
        self.name = name
        self.w = None
        self.r = []

    def __getitem__(self, k):
        return V(self.t[k], self)

    @property
    def all(self):
        return V(self.t[:], self)


class V:
    def __init__(self, ap, owner):
        self.ap = ap
        self.o = owner

    def __getitem__(self, k):
        return V(self.ap[k], self.o)

    def rr(self, pat, **kw):
        return V(self.ap.rearrange(pat, **kw), self.o)

    def bc(self, shape):
        return V(self.ap.to_broadcast(shape), self.o)

    def bitcast(self, dt):
        return V(self.ap.bitcast(dt), self.o)


class Prog:
    def __init__(self, name="p"):
        self.name = name
        self.nc = bass.Bass("TRN2", target_bir_lowering=False)
        self.es = ExitStack()
        self.ops = []
        self.eng = {"pe": self.nc.tensor, "dve": self.nc.vector, "act": self.nc.scalar,
                    "pool": self.nc.gpsimd, "sp": self.nc.sync}
        self.outs = []
        self._n = 0

    def _nm(self, name):
        self._n += 1
        return f"{name}_{self._n}"

    def din(self, name, shape, dt=F32):
        return T(self.nc.dram_tensor(name, list(shape), dt, kind="ExternalInput").ap(), name)

    def dout(self, name, shape, dt=F32):
        t = T(self.nc.dram_tensor(name, list(shape), dt, kind="ExternalOutput").ap(), name)
        self.outs.append(t)
        return t

    def dtmp(self, name, shape, dt=F32):
        return T(self.nc.dram_tensor(name, list(shape), dt, kind="Internal").ap(), name)

    def sb(self, name, shape, dt=F32):
        nm = self._nm(name)
        return T(self.es.enter_context(self.nc.sbuf_tensor(nm, list(shape), dt)), nm)

    def ps(self, name, shape, dt=F32):
        nm = self._nm(name)
        return T(self.es.enter_context(self.nc.psum_tensor(nm, list(shape), dt)), nm)

    def I(self, eng, build, w=(), r=(), dma=False):
        self.ops.append((eng, build, [x.o if isinstance(x, V) else x for x in w],
                         [x.o if isinstance(x, V) else x for x in r], dma))

    def dma(self, q, out, in_, **kw):
        if getattr(self, "force_sp", False):
            q = "sp"
        e = self.eng[q]
        self.I(q, lambda: e.dma_start(out=out.ap, in_=in_.ap, **kw), w=[out], r=[in_], dma=True)

    def mm(self, out, lhsT, rhs, start=True, stop=True, **kw):
        self.I("pe", lambda: self.nc.tensor.matmul(out.ap, lhsT.ap, rhs.ap, start=start, stop=stop, **kw),
               w=[out], r=[lhsT, rhs])

    def tr(self, out, in_, ident):
        self.I("pe", lambda: self.nc.tensor.transpose(out.ap, in_.ap, ident.ap), w=[out], r=[in_, ident])

    def act(self, out, in_, func, bias=None, scale=None, accum=None, eng="act"):
        kw = {}
        r = [in_]
        w = [out]
        if bias is not None:
            kw["bias"] = bias.ap if isinstance(bias, V) else bias
            if isinstance(bias, V):
                r.append(bias)
        if scale is not None:
            kw["scale"] = scale.ap if isinstance(scale, V) else scale
            if isinstance(scale, V):
                r.append(scale)
        if accum is not None:
            kw["accum_out"] = accum.ap
            w.append(accum)
        self.I("act", lambda: self.nc.scalar.activation(out=out.ap, in_=in_.ap, func=func, **kw), w=w, r=r)

    def tt(self, eng, out, a, b, op):
        e = self.eng[eng]
        self.I(eng, lambda: e.tensor_tensor(out=out.ap, in0=a.ap, in1=b.ap, op=op), w=[out], r=[a, b])

    def ts(self, eng, out, a, s1, s2=None, op0=ALU.mult, op1=None, accum=None):
        e = self.eng[eng]
        r = [a]
        w = [out]
        v1 = s1.ap if isinstance(s1, V) else s1
        v2 = s2.ap if isinstance(s2, V) else s2
        if isinstance(s1, V):
            r.append(s1)
        if isinstance(s2, V):
            r.append(s2)
        kw = {}
        if op1 is not None:
            kw["op1"] = op1
        if accum is not None:
            kw["accum_out"] = accum.ap
            w.append(accum)
        self.I(eng, lambda: e.tensor_scalar(out=out.ap, in0=a.ap, scalar1=v1, scalar2=v2, op0=op0, **kw), w=w, r=r)

    def stt(self, out, a, s, b, op0, op1, eng="dve"):
        e = self.eng[eng]
        r = [a, b]
        sv = s.ap if isinstance(s, V) else s
        if isinstance(s, V):
            r.append(s)
        self.I(eng, lambda: e.scalar_tensor_tensor(out=out.ap, in0=a.ap, scalar=sv, in1=b.ap, op0=op0, op1=op1),
               w=[out], r=r)

    def copy(self, eng, out, in_):
        if eng == "act":
            self.I("act", lambda: self.nc.scalar.copy(out=out.ap, in_=in_.ap), w=[out], r=[in_])
        else:
            e = self.eng[eng]
            self.I(eng, lambda: e.tensor_copy(out=out.ap, in_=in_.ap), w=[out], r=[in_])

    def memset(self, eng, out, val):
        e = self.eng[eng]
        self.I(eng, lambda: e.memset(out.ap, val), w=[out])

    def reduce(self, out, in_, op, axis=AX.X, eng="dve"):
        e = self.eng[eng]
        self.I(eng, lambda: e.tensor_reduce(out=out.ap, in_=in_.ap, axis=axis, op=op), w=[out], r=[in_])

    def recip(self, out, in_):
        self.I("dve", lambda: self.nc.vector.reciprocal(out=out.ap, in_=in_.ap), w=[out], r=[in_])

    def finish(self):
        nc = self.nc
        ops = self.ops
        n = len(ops)
        deps = [None] * n
        needed = [False] * n
        engs = list(self.eng.keys())
        known = {e: {e2: -1 for e2 in engs} for e in engs}
        known_dma = {e: set() for e in engs}
        last_dma_on_sem = [None] * NDMASEM
        ndma = 0
        dma_slot = {}
        for i, (eng, build, w, r, isdma) in enumerate(ops):
            d = set()
            for b in r:
                if b.w is not None:
                    d.add(b.w)
            for b in w:
                if b.w is not None:
                    d.add(b.w)
                for x in b.r:
                    d.add(x)
            if isdma:
                s = ndma % NDMASEM
                dma_slot[i] = (s, 16 * (ndma // NDMASEM + 1))
                if last_dma_on_sem[s] is not None:
                    d.add(last_dma_on_sem[s])
                last_dma_on_sem[s] = i
                ndma += 1
            d.discard(i)
            keep_c = {}
            keep_d = []
            for j in d:
                e2, _, _, _, jd = ops[j]
                if jd:
                    if j not in known_dma[eng]:
                        keep_d.append(j)
                        known_dma[eng].add(j)
                else:
                    if e2 == "pe" and eng == "pe":
                        continue
                    if j > known[eng][e2]:
                        keep_c[e2] = max(keep_c.get(e2, -1), j)
            for e2, j in keep_c.items():
                known[eng][e2] = j
            deps[i] = (list(keep_c.values()), keep_d)
            for j in deps[i][0]:
                needed[j] = True
            for b in r:
                b.r.append(i)
            for b in w:
                b.w = i
                b.r = []
        sems = {e: self.es.enter_context(nc.semaphore(f"s_{e}")) for e in engs}
        dsems = [self.es.enter_context(nc.semaphore(f"d_{k}")) for k in range(NDMASEM)]
        cnt = {e: 0 for e in engs}
        val = [None] * n
        for i, (eng, build, w, r, isdma) in enumerate(ops):
            e = self.eng[eng]
            for j in deps[i][0]:
                e.wait_ge(sems[ops[j][0]], val[j])
            for j in deps[i][1]:
                s, v = dma_slot[j]
                e.wait_ge(dsems[s], v)
            ins = build()
            if isdma:
                s, v = dma_slot[i]
                ins.then_inc(dsems[s], 16)
            elif needed[i]:
                cnt[eng] += 1
                val[i] = cnt[eng]
                ins.then_inc(sems[eng], 1)
        for s in range(NDMASEM):
            j = last_dma_on_sem[s]
            if j is not None:
                nc.sync.wait_ge(dsems[s], dma_slot[j][1])
        self.nops = n
        return nc


import os as _os
import time as _time
_T0 = [None]


def run(prog_nc, in_maps, ncores=8):
    t = _time.time()
    res = run_bass_kernel_spmd(prog_nc, in_maps, core_ids=list(range(ncores)))
    if _os.environ.get("KDEBUG"):
        if _T0[0] is None:
            _T0[0] = t
        print(f"[run] {_time.time() - t:7.1f}s  total {_time.time() - _T0[0]:7.1f}s  outs={list(res.results[0].keys())}", flush=True)
    return res.results


D = 4096
NCORE = 8


def build_adaln():
    P = Prog("adaln")
    NCOL = 6 * D // NCORE
    cT = P.din("cT", [128, 32, 2])
    W = P.din("W", [2, D, NCOL])
    B = P.din("B", [2, 2, NCOL])
    O = P.dout("O", [2, 2, NCOL])
    c_sb = P.sb("c", [128, 32, 2])
    s_sb = P.sb("s", [128, 32, 2])
    b_sb = P.sb("b", [2, 2, NCOL])
    o_sb = P.sb("o", [2, 2, NCOL])
    wt = [P.sb("w", [128, NCOL]) for _ in range(4)]
    ps = [P.ps("ps", [2, 512]) for _ in range(6)]
    P.dma("sp", c_sb.all, cT.all)
    P.dma("sp", b_sb.all, B.all)
    P.act(s_sb.all, c_sb.all, AF.Silu)
    k = 0
    for l in range(2):
        for kc in range(32):
            w = wt[k % 4]
            k += 1
            P.dma("sp" if k % 2 else "pool", w.all, W[l, kc * 128:(kc + 1) * 128, :])
            for nt in range(6):
                P.mm(ps[nt].all, s_sb[:, kc, :], w[:, nt * 512:(nt + 1) * 512], start=(kc == 0), stop=(kc == 31))
        for nt in range(6):
            P.tt("dve", o_sb[:, l, nt * 512:(nt + 1) * 512], ps[nt].all, b_sb[:, l, nt * 512:(nt + 1) * 512], ALU.add)
    P.dma("sp", O.all.rr("l m n -> m l n"), o_sb.all)
    return P.finish()


def run_adaln(c, c_ctx, ada_w, ada_b):
    NCOL = 6 * D // NCORE
    c2 = np.stack([c.reshape(D), c_ctx.reshape(D)], 0)
    cT = np.ascontiguousarray(c2.reshape(2, 32, 128).transpose(2, 1, 0))
    maps = []
    for i in range(NCORE):
        sl = slice(i * NCOL, (i + 1) * NCOL)
        Bm = np.ascontiguousarray(np.broadcast_to(ada_b[None, :, sl], (2, 2, NCOL)))
        maps.append({"cT": cT, "W": np.ascontiguousarray(ada_w[:, :, sl]), "B": Bm})
    nc = build_adaln()
    res = run(nc, maps)
    return np.concatenate([r["O"] for r in res], axis=-1)


EPS = 1e-6


def build_rowops(NT, NL, nparts, router, want_h=True, want_u=True, udt=BF16, pdt=F32):
    P = Prog("rowops")
    h = P.din("h", [NT, 128, D])
    parts = [P.din(f"y{i}", [NT, 128, D], pdt) for i in range(nparts)]
    vec = P.din("vec", [2, 4, D])
    hn = P.dout("hn", [NT, 128, D]) if want_h else None
    uo = P.dout("u", [NT, 128, D], udt) if want_u else None
    if router:
        rw = P.din("rw", [128, 32, 16])
        ident = P.din("ident", [128, 128])
        aff = P.dout("aff", [NT, 128, 16])
        rw_sb = P.sb("rw", [128, 32, 16])
        id_sb = P.sb("id", [128, 128])
        P.dma("sp", rw_sb.all, rw.all)
        P.dma("sp", id_sb.all, ident.all)
        u2T = [P.sb("u2T", [128, 32, 128]) for _ in range(1)]
        pst = [P.ps("pst", [128, 512]) for _ in range(4)]
        psl = [P.ps("psl", [128, 16]) for _ in range(2)]
    vg = P.sb("vg", [128, D])
    va = P.sb("va", [128, D])
    vs = P.sb("vs", [128, D])
    ht = [P.sb("ht", [128, D]) for _ in range(2)]
    yt = [P.sb("yt", [128, D], pdt) for _ in range(1)]
    yf = P.sb("yf", [128, D]) if pdt != F32 else None
    tmp = [P.sb("tmp", [128, D]) for _ in range(1)]
    sq = tmp[0]
    ut = [P.sb("ut", [128, D], udt) for _ in range(2)]
    st = [P.sb("st", [128, 8]) for _ in range(2)]
    lgs = [P.sb("lg", [128, 16]) for _ in range(2)]

    def load_vecs(kd):
        P.dma("sp", vg.all, vec[kd, 0:1, :].bc([128, D]))
        P.dma("sp", va.all, vec[kd, 1:2, :].bc([128, D]))
        P.dma("sp", vs.all, vec[kd, 2:3, :].bc([128, D]))
        P.dma("sp", tmp[0].all, vec[kd, 3:4, :].bc([128, D]))
        P.stt(va.all, va.all, 1.0, tmp[0].all, ALU.add, ALU.mult)
    vb = {(k, "g"): vg for k in range(2)}
    vb.update({(k, "sc"): va for k in range(2)})
    vb.update({(k, "sh"): vs for k in range(2)})
    for i in range(NT):
        kd = 0 if i < NL else 1
        if i == 0 or i == NL:
            load_vecs(kd)
        hb = ht[i % 2]
        P.dma("sp", hb.all, h[i])
        for p in range(nparts):
            yb = yt[0]
            P.dma("pool", yb.all, parts[p][i])
            yo_ = yb if yf is None else yf
            P.tt("pool", yo_.all, yb.all, vb[(kd, "g")].all, ALU.mult)
            P.tt("dve", hb.all, hb.all, yo_.all, ALU.add)
        if want_h:
            P.dma("pool", hn[i], hb.all)
        s = st[i % 2]
        P.act(sq.all, hb.all, AF.Square, accum=s[:, 0:1])
        P.ts("dve", s[:, 1:2], s[:, 0:1], 1.0 / D, EPS, op0=ALU.mult, op1=ALU.add)
        P.act(s[:, 7:8], s[:, 1:2], AF.Sqrt)
        P.recip(s[:, 2:3], s[:, 7:8])
        tb = tmp[0]
        P.stt(tb.all, hb.all, s[:, 2:3], vb[(kd, "sc")].all, ALU.mult, ALU.mult)
        if router:
            P.tt("dve", tb.all, tb.all, vb[(kd, "sh")].all, ALU.add)
            if want_u:
                P.copy("pool", ut[i % 2].all, tb.all)
        else:
            P.tt("dve", ut[i % 2].all, tb.all, vb[(kd, "sh")].all, ALU.add)
        if want_u:
            P.dma("sp", uo[i], ut[i % 2].all)
        if router:
            uT = u2T[0]
            for g4 in range(8):
                pt = pst[g4 % 4]
                for j in range(4):
                    kc = g4 * 4 + j
                    P.tr(pt[:, j * 128:(j + 1) * 128], tb[:, kc * 128:(kc + 1) * 128], id_sb.all)
                P.copy("act" if g4 % 2 else "dve", uT[:, g4 * 4:(g4 + 1) * 4, :],
                       pt.all.rr("p (a b) -> p a b", a=4))
            pl = psl[i % 2]
            for kc in range(32):
                P.mm(pl.all, uT[:, kc, :], rw_sb[:, kc, :], start=(kc == 0), stop=(kc == 31))
            lg = lgs[i % 2]
            P.copy("dve", lg.all, pl.all)
            P.reduce(s[:, 3:4], lg.all, ALU.max)
            P.ts("dve", s[:, 4:5], s[:, 3:4], -1.0, None, op0=ALU.mult)
            P.act(lg.all, lg.all, AF.Exp, bias=s[:, 4:5], scale=1.0, accum=s[:, 5:6])
            P.recip(s[:, 6:7], s[:, 5:6])
            P.ts("dve", lg.all, lg.all, s[:, 6:7], None, op0=ALU.mult)
            P.dma("pool", aff[i], lg.all)
    return P.finish()


def build_gemm(KC, NTOK, nblocks, TB=512):
    P = Prog("gemm")
    NTOT = sum(w for w, _ in nblocks)
    AT = P.din("AT", [128, KC, NTOK], BF16)
    W = P.din("W", [KC * 128, NTOT])
    outs = []
    for bi, (wd, kind) in enumerate(nblocks):
        outs.append(P.dout(f"O{bi}", [NTOK, wd], F32 if kind == "f32" else BF16))
    maxw = max(w for w, _ in nblocks)
    wb = P.sb("wb", [128, KC, maxw], BF16)
    stg = [P.sb("stg", [128, maxw]) for _ in range(2)]
    ab = [P.sb("ab", [128, KC, TB], BF16) for _ in range(2)]
    ob = {k: [P.sb("ob" + k, [128, maxw], F32 if k == "f32" else BF16) for _ in range(2)] for k in set(k for _, k in nblocks)}
    ps = [P.ps("ps", [128, 512]) for _ in range(6)]
    n0 = 0
    pi = 0
    ai = 0
    oi = 0
    for bi, (wd, kind) in enumerate(nblocks):
        for kc in range(KC):
            s = stg[kc % 2]
            P.dma("sp" if kc % 2 else "pool", s[:, :wd], W[kc * 128:(kc + 1) * 128, n0:n0 + wd])
            P.copy("dve" if kc % 2 else "pool", wb[:, kc, :wd], s[:, :wd])
        ntiles = [(a, min(512, wd - a)) for a in range(0, wd, 512)]
        for t0 in range(0, NTOK, TB):
            tb = min(TB, NTOK - t0)
            a = ab[ai % 2]
            ai += 1
            P.dma("sp", a[:, :, :tb], AT[:, :, t0:t0 + tb])
            for tt in range(0, tb, 128):
                o = ob[kind][oi % 2]
                oi += 1
                for (na, nw) in ntiles:
                    p = ps[pi % 6]
                    pi += 1
                    for kc in range(KC):
                        P.mm(p[:, :nw], a[:, kc, tt:tt + 128], wb[:, kc, na:na + nw], start=(kc == 0), stop=(kc == KC - 1))
                    if kind == "sig_bf16":
                        P.act(o[:, na:na + nw], p[:, :nw], AF.Sigmoid)
                    else:
                        P.copy("act" if (pi % 2) else "dve", o[:, na:na + nw], p[:, :nw])
                P.dma("pool", outs[bi][t0 + tt:t0 + tt + 128, :], o[:, :wd])
        n0 += wd
    return P.finish()


GRID_W = 64


def build_na(ROWS, NCTX, ctx_out=True):
    P = Prog("na")
    NLAT = ROWS * 64
    NT = NLAT + NCTX
    NC64 = NCTX // 64
    qT = P.din("qT", [128, NT])
    kT = P.din("kT", [128, NT])
    v = P.din("v", [64, ROWS + NC64, 128])
    bias = P.din("bias", [64, 15 * 64])
    ident = P.din("ident", [64, 64])
    o = P.dout("o", [NT, 128], BF16)
    scale = 128 ** -0.5
    q_sb = P.sb("q", [128, NT], BF16)
    k_sb = P.sb("k", [128, NT], BF16)
    v_sb = P.sb("v", [64, ROWS + NC64, 128], BF16)
    b_sb = P.sb("b", [64, 15 * 64])
    id_f = P.sb("idf", [64, 64])
    id_sb = P.sb("id", [64, 64], BF16)
    P.dma("sp", b_sb.all, bias.all)
    P.dma("sp", id_f.all, ident.all)
    P.copy("dve", id_sb.all, id_f.all)
    stg = [P.sb("stg", [128, 2048]) for _ in range(2)]
    k = 0
    for src, dst in ((qT, q_sb), (kT, k_sb)):
        for c0 in range(0, NT, 2048):
            cw = min(2048, NT - c0)
            s = stg[k % 2]
            P.dma("sp" if k % 2 else "pool", s[:, :cw], src[:, c0:c0 + cw])
            P.copy("dve" if k % 2 else "pool", dst[:, c0:c0 + cw], s[:, :cw])
            k += 1
    for r0 in range(0, ROWS + NC64, 16):
        rw = min(16, ROWS + NC64 - r0)
        s = stg[k % 2]
        sv = s[0:64, :].rr("p (a b) -> p a b", b=128)
        P.dma("sp" if k % 2 else "pool", sv[:, :rw, :], v[:, r0:r0 + rw, :])
        P.copy("dve" if k % 2 else "pool", v_sb[:, r0:r0 + rw, :], sv[:, :rw, :])
        k += 1
    NKMAX = 512 + NCTX
    ps_s = [P.ps("pss", [64, 512]) for _ in range(2)]
    ps_c = [P.ps("psc", [64, NCTX]) for _ in range(2)]
    ps_t = [P.ps("pst", [64, NKMAX], BF16) for _ in range(2)]
    ps_o = [P.ps("pso", [64, 128]) for _ in range(2)]
    S = [P.sb("S", [64, NKMAX]) for _ in range(2)]
    Pb = [P.sb("Pb", [64, NKMAX], BF16) for _ in range(2)]
    PT = [P.sb("PT", [64, NKMAX], BF16) for _ in range(2)]
    st = [P.sb("st", [64, 4]) for _ in range(2)]
    ob = [P.sb("ob", [64, 128], BF16) for _ in range(2)]
    wr = min(8, ROWS)
    steps = [("lat", r) for r in range(ROWS)]
    if ctx_out:
        steps += [("ctx", j) for j in range(NC64)]
    for it, (kind, r) in enumerate(steps):
        b = it % 2
        if kind == "lat":
            rs = min(max(r - wr // 2, 0), ROWS - wr)
            dr0 = rs - r + 7
            qs = q_sb[:, r * 64:(r + 1) * 64]
            P.mm(ps_s[b][:, :wr * 64], qs, k_sb[:, rs * 64:(rs + wr) * 64])
            P.mm(ps_c[b].all, qs, k_sb[:, NLAT:NT])
            P.stt(S[b][:, :wr * 64], ps_s[b][:, :wr * 64], scale, b_sb[:, dr0 * 64:(dr0 + wr) * 64], ALU.mult, ALU.add)
            P.ts("dve", S[b][:, wr * 64:wr * 64 + NCTX], ps_c[b].all, scale, None, op0=ALU.mult)
            nk = wr * 64 + NCTX
            vrows = list(range(rs, rs + wr)) + list(range(ROWS, ROWS + NC64))
            orow = r * 64
        else:
            qs = q_sb[:, NLAT + r * 64:NLAT + (r + 1) * 64]
            P.mm(ps_c[b].all, qs, k_sb[:, NLAT:NT])
            P.ts("dve", S[b][:, :NCTX], ps_c[b].all, scale, None, op0=ALU.mult)
            nk = NCTX
            vrows = list(range(ROWS, ROWS + NC64))
            orow = NLAT + r * 64
        s = st[b]
        P.reduce(s[:, 0:1], S[b][:, :nk], ALU.max)
        P.ts("dve", s[:, 1:2], s[:, 0:1], -1.0, None, op0=ALU.mult)
        P.act(Pb[b][:, :nk], S[b][:, :nk], AF.Exp, bias=s[:, 1:2], scale=1.0, accum=s[:, 2:3])
        P.recip(s[:, 3:4], s[:, 2:3])
        for j in range(nk // 64):
            P.tr(ps_t[b][:, j * 64:(j + 1) * 64], Pb[b][:, j * 64:(j + 1) * 64], id_sb.all)
        P.copy("act", PT[b][:, :nk], ps_t[b][:, :nk])
        for j, vr in enumerate(vrows):
            P.mm(ps_o[b].all, PT[b][:, j * 64:(j + 1) * 64], v_sb[:, vr, :], start=(j == 0), stop=(j == len(vrows) - 1))
        P.ts("dve", ob[b].all, ps_o[b].all, s[:, 3:4], None, op0=ALU.mult)
        P.dma("sp" if b else "pool", o[orow:orow + 64, :], ob[b].all)
    return P.finish()


def na_bias_table(rpb_h):
    q = np.arange(64)[:, None]
    kk = np.arange(64)[None, :]
    dc = np.clip(kk - q + 15, 0, 30)
    tab = rpb_h[:, dc]
    tab = np.ascontiguousarray(tab.transpose(1, 0, 2))
    c0 = np.clip(q - 8, 0, 48)
    ok = (kk >= c0) & (kk < c0 + 16)
    out = np.where(ok[:, None, :], tab, np.float32(-30000.0)).astype(np.float32)
    return out.reshape(64, 15 * 64)


def build_mla(NLAT, NCTX, ctx_out=True):
    P = Prog("mla")
    NT = NLAT + NCTX
    TB = 256
    cqT = P.din("cqT", [128, 8, NT])
    ckvT = P.din("ckvT", [128, 4, NT])
    krT = P.din("krT", [64, NT])
    krsT = P.din("krsT", [64, NT])
    nwq = P.din("nwq", [128, 8])
    nwkv = P.din("nwkv", [128, 4])
    wuq = P.din("wuq", [128, 8, 192])
    wuqs = P.din("wuqs", [128, 8, 64])
    wukv = P.din("wukv", [128, 4, 256])
    cos = P.din("cos", [64, NLAT])
    sins = P.din("sins", [64, NLAT])
    ident = P.din("ident", [128, 128])
    o = P.dout("o", [NT, 128], BF16)
    scale = 192 ** -0.5
    nwq_sb = P.sb("nwq", [128, 8]); P.dma("sp", nwq_sb.all, nwq.all)
    nwkv_sb = P.sb("nwkv", [128, 4]); P.dma("sp", nwkv_sb.all, nwkv.all)
    wst = P.sb("wst", [128, 8 * 192])
    wuq_sb = P.sb("wuq", [128, 8, 192], BF16)
    wuqs_sb = P.sb("wuqs", [128, 8, 64], BF16)
    wukv_sb = P.sb("wukv", [128, 4, 256], BF16)
    P.dma("sp", wst[:, :8 * 192], wuq.all.rr("p a b -> p (a b)"))
    P.copy("dve", wuq_sb.all.rr("p a b -> p (a b)"), wst[:, :8 * 192])
    P.dma("sp", wst[:, :8 * 64], wuqs.all.rr("p a b -> p (a b)"))
    P.copy("dve", wuqs_sb.all.rr("p a b -> p (a b)"), wst[:, :8 * 64])
    P.dma("sp", wst[:, :4 * 256], wukv.all.rr("p a b -> p (a b)"))
    P.copy("dve", wukv_sb.all.rr("p a b -> p (a b)"), wst[:, :4 * 256])
    idf = P.sb("idf", [128, 128]); P.dma("sp", idf.all, ident.all)
    id_sb = P.sb("id", [128, 128], BF16); P.copy("dve", id_sb.all, idf.all)
    ones = P.sb("ones", [128, 128], BF16); P.memset("dve", ones.all, 1.0)
    qnT = P.sb("qnT", [128, NT], BF16)
    qrT = P.sb("qrT", [64, NT], BF16)
    knT = P.sb("knT", [128, NT], BF16)
    krT_sb = P.sb("krT", [64, NT], BF16)
    v_sb = P.sb("v", [128, NT // 128, 128], BF16)
    cq_s = P.sb("cqs", [128, 8, TB])
    sq = P.sb("sq", [128, 8, TB], BF16)
    cqn = P.sb("cqn", [128, 8, TB], BF16)
    ckv_s = P.sb("ckvs", [128, 4, TB])
    ckvn = P.sb("ckvn", [128, 4, TB], BF16)
    rq = P.sb("rq", [128, TB])
    rt = P.sb("rt", [128, TB])
    cs = [P.sb("cs", [64, TB]) for _ in range(2)]
    kr_s = [P.sb("krs", [64, TB]) for _ in range(2)]
    t1 = P.sb("t1", [64, TB]); t2 = P.sb("t2", [64, TB])
    pf = [P.ps("pf", [128, 512]) for _ in range(6)]
    pb = [P.ps("pb", [128, 512], BF16) for _ in range(2)]

    def rms(ps, src, dst, nw_sb, nkc, dim):
        P.act(sq[:, :nkc, :tb], src[:, :nkc, :tb], AF.Square)
        for kc in range(nkc):
            P.mm(ps[:, :tb], ones.all, sq[:, kc, :tb], start=(kc == 0), stop=(kc == nkc - 1))
        P.ts("dve", rt[:, :tb], ps[:, :tb], 1.0 / dim, EPS, op0=ALU.mult, op1=ALU.add)
        P.act(rt[:, :tb], rt[:, :tb], AF.Sqrt)
        P.recip(rq[:, :tb], rt[:, :tb])
        for kc in range(nkc):
            P.stt(dst[:, kc, :tb], src[:, kc, :tb], nw_sb[:, kc:kc + 1], rq[:, :tb], ALU.mult, ALU.mult)

    for t0 in range(0, NT, TB):
        tb = min(TB, NT - t0)
        lat = t0 < NLAT
        P.dma("sp", cq_s[:, :, :tb], cqT[:, :, t0:t0 + tb])
        P.dma("pool", ckv_s[:, :, :tb], ckvT[:, :, t0:t0 + tb])
        P.dma("sp", kr_s[0][:, :tb], krT[:, t0:t0 + tb])
        if lat:
            P.dma("pool", kr_s[1][:, :tb], krsT[:, t0:t0 + tb])
            P.dma("sp", cs[0][:, :tb], cos[:, t0:t0 + tb])
            P.dma("pool", cs[1][:, :tb], sins[:, t0:t0 + tb])
        rms(pf[0], cq_s, cqn, nwq_sb, 8, 1024)
        for kc in range(8):
            P.mm(pf[1][:, :tb], wuq_sb[:, kc, 0:128], cqn[:, kc, :tb], start=(kc == 0), stop=(kc == 7))
        P.copy("act", qnT[:, t0:t0 + tb], pf[1][:, :tb])
        for kc in range(8):
            P.mm(pf[2][0:64, :tb], wuq_sb[:, kc, 128:192], cqn[:, kc, :tb], start=(kc == 0), stop=(kc == 7))
        if lat:
            for kc in range(8):
                P.mm(pf[3][0:64, :tb], wuqs_sb[:, kc, :], cqn[:, kc, :tb], start=(kc == 0), stop=(kc == 7))
            P.tt("dve", t1[:, :tb], pf[2][0:64, :tb], cs[0][:, :tb], ALU.mult)
            P.tt("dve", t2[:, :tb], pf[3][0:64, :tb], cs[1][:, :tb], ALU.mult)
            P.tt("dve", qrT[:, t0:t0 + tb], t1[:, :tb], t2[:, :tb], ALU.add)
            P.tt("pool", t1[:, :tb], kr_s[0][:, :tb], cs[0][:, :tb], ALU.mult)
            P.tt("pool", t2[:, :tb], kr_s[1][:, :tb], cs[1][:, :tb], ALU.mult)
            P.tt("pool", krT_sb[:, t0:t0 + tb], t1[:, :tb], t2[:, :tb], ALU.add)
        else:
            P.copy("dve", qrT[:, t0:t0 + tb], pf[2][0:64, :tb])
            P.copy("pool", krT_sb[:, t0:t0 + tb], kr_s[0][:, :tb])
        rms(pf[0], ckv_s, ckvn, nwkv_sb, 4, 512)
        for kc in range(4):
            P.mm(pf[4][:, :tb], wukv_sb[:, kc, 0:128], ckvn[:, kc, :tb], start=(kc == 0), stop=(kc == 3))
        P.copy("act", knT[:, t0:t0 + tb], pf[4][:, :tb])
        for j in range(tb // 128):
            for kc in range(4):
                P.mm(pf[5][:, j * 128:(j + 1) * 128], ckvn[:, kc, j * 128:(j + 1) * 128], wukv_sb[:, kc, 128:256],
                     start=(kc == 0), stop=(kc == 3))
        P.copy("dve", v_sb[:, t0 // 128:t0 // 128 + tb // 128, :], pf[5][:, :tb].rr("p (a b) -> p a b", b=128))
    S = P.sb("S", [128, NT])
    Pb = P.sb("Pb", [128, NT], BF16)
    PT = P.sb("PT", [128, NT], BF16)
    st = [P.sb("st", [128, 4]) for _ in range(2)]
    ob = [P.sb("ob", [128, 128], BF16) for _ in range(2)]
    steps = [("lat", i) for i in range(NLAT // 128)]
    if ctx_out:
        steps += [("ctx", i) for i in range(NCTX // 128)]
    pi = 0
    for it, (kind, i) in enumerate(steps):
        if kind == "lat":
            q0 = i * 128; k0 = 0; nk = NT
        else:
            q0 = NLAT + i * 128; k0 = NLAT; nk = NCTX
        for c0 in range(0, nk, 512):
            cw = min(512, nk - c0)
            p = pf[pi % 2]; pi += 1
            P.mm(p[:, :cw], qnT[:, q0:q0 + 128], knT[:, k0 + c0:k0 + c0 + cw], start=True, stop=False)
            P.mm(p[:, :cw], qrT[:, q0:q0 + 128], krT_sb[:, k0 + c0:k0 + c0 + cw], start=False, stop=True)
            if pi % 2:
                P.ts("dve", S[:, c0:c0 + cw], p[:, :cw], scale, None, op0=ALU.mult)
            else:
                P.act(S[:, c0:c0 + cw], p[:, :cw], AF.Copy, scale=scale)
        s = st[it % 2]
        P.reduce(s[:, 0:1], S[:, :nk], ALU.max)
        P.ts("dve", s[:, 1:2], s[:, 0:1], -1.0, None, op0=ALU.mult)
        P.act(Pb[:, :nk], S[:, :nk], AF.Exp, bias=s[:, 1:2], scale=1.0, accum=s[:, 2:3])
        P.recip(s[:, 3:4], s[:, 2:3])
        for c0 in range(0, nk, 512):
            cw = min(512, nk - c0)
            p = pb[(c0 // 512) % 2]
            for j in range(cw // 128):
                P.tr(p[:, j * 128:(j + 1) * 128], Pb[:, c0 + j * 128:c0 + (j + 1) * 128], id_sb.all)
            P.copy("act" if (c0 // 512) % 2 else "dve", PT[:, c0:c0 + cw], p[:, :cw])
        po = pf[2 + it % 2]
        nj = nk // 128
        for j in range(nj):
            P.mm(po[:, 0:128], PT[:, j * 128:(j + 1) * 128], v_sb[:, k0 // 128 + j, :], start=(j == 0), stop=(j == nj - 1))
        P.ts("dve", ob[it % 2].all, po[:, 0:128], s[:, 3:4], None, op0=ALU.mult)
        P.dma("sp" if it % 2 else "pool", o[q0:q0 + 128, :], ob[it % 2].all)
    return P.finish()


def rope_tables(NLAT):
    t = np.arange(NLAT)
    row, col = t // GRID_W, t % GRID_W
    nf = 16
    inv = (10000.0 ** (-np.arange(nf, dtype=np.float32) / nf)).astype(np.float32)
    cos = np.zeros((64, NLAT), np.float32); sins = np.zeros((64, NLAT), np.float32)
    for base, p in ((0, row), (32, col)):
        ang = p.astype(np.float32)[None, :] * inv[:, None]
        c, s = np.cos(ang).astype(np.float32), np.sin(ang).astype(np.float32)
        cos[base:base + 16] = c; cos[base + 16:base + 32] = c
        sins[base:base + 16] = -s; sins[base + 16:base + 32] = s
    return cos, sins


SWAP64 = np.concatenate([np.arange(16, 32), np.arange(0, 16), np.arange(48, 64), np.arange(32, 48)])


def build_gla(NLAT, NCTX):
    P = Prog("gla")
    NT = NLAT + NCTX
    NCH = NT // 64
    SBK = 2048
    qT = P.din("qT", [2, 64, NT]); kT = P.din("kT", [2, 64, NT])
    v = P.din("v", [2, 64, NCH, 128]); lrT = P.din("lrT", [2, 16, NT])
    w2 = P.din("w2", [2, 16, 64]); b2 = P.din("b2", [64, 2])
    g = P.din("g", [NT, 128]); nw = P.din("nw", [128, 128])
    m01 = P.din("m01", [64, SBK]); tri = P.din("tri", [64, 64]); id64 = P.din("id64", [64, 64])
    J = P.din("J", [128, 128])
    o = P.dout("o", [NT, 128], BF16)
    od = [P.dtmp(f"od{d}", [NT, 128]) for d in range(2)]
    m01_sb = P.sb("m01", [64, SBK]); P.dma("sp", m01_sb.all, m01.all)
    tri_sb = P.sb("tri", [64, 64]); P.dma("sp", tri_sb.all, tri.all)
    id_sb = P.sb("id", [64, 64]); P.dma("sp", id_sb.all, id64.all)
    J_sb = P.sb("J", [128, 128]); P.dma("sp", J_sb.all, J.all)
    nw_sb = P.sb("nw", [128, 128]); P.dma("sp", nw_sb.all, nw.all)
    w2_sb = P.sb("w2", [16, 2, 64]); P.dma("sp", w2_sb.all, w2.all.rr("d r c -> r d c"))
    b2_sb = P.sb("b2", [64, 2]); P.dma("sp", b2_sb.all, b2.all)
    nb2 = P.sb("nb2", [64, 2]); P.ts("dve", nb2.all, b2_sb.all, -1.0, None, op0=ALU.mult)
    q_s = P.sb("q", [64, SBK]); k_s = P.sb("k", [64, SBK]); lr_s = P.sb("lr", [16, SBK])
    v_s = P.sb("vs", [64, SBK // 64, 128]); v_b = P.sb("vb", [64, SBK // 64, 128], BF16)
    e1 = P.sb("e1", [64, SBK]); cum = P.sb("cum", [64, SBK]); d1 = P.sb("d1", [64, SBK]); E = P.sb("E", [64, SBK])
    qe = P.sb("qe", [64, SBK], BF16); ke = P.sb("ke", [64, SBK], BF16); qs = P.sb("qs", [64, SBK], BF16)
    kdT = P.sb("kdT", [64, SBK]); kd = P.sb("kd", [64, SBK // 64, 64], BF16)
    dec = P.sb("dec", [64, SBK // 64])
    S = P.sb("S", [64, 128]); S_b = P.sb("Sb", [64, 128], BF16)
    sT = [P.sb("sT", [64, 64], BF16) for _ in range(2)]
    o8 = [P.sb("o8", [64, 8, 128]) for _ in range(2)]
    pz = P.ps("pz", [64, 512]); ptr = P.ps("ptr", [64, 512])
    pst = [P.ps("pst", [64, 64]) for _ in range(2)]
    po = [P.ps("po", [64, 128]) for _ in range(2)]
    pds = P.ps("pds", [64, 128]); pfin = P.ps("pfin", [128, 128])
    for d in range(2):
        P.memset("dve", S.all, 0.0)
        P.memset("dve", S_b.all, 0.0)
        for t0 in range(0, NT, SBK):
            tb = min(SBK, NT - t0)
            nch = tb // 64
            P.dma("sp", q_s[:, :tb], qT[d, :, t0:t0 + tb])
            P.dma("pool", k_s[:, :tb], kT[d, :, t0:t0 + tb])
            P.dma("sp", lr_s[:, :tb], lrT[d, :, t0:t0 + tb])
            P.dma("pool", v_s[:, :nch, :], v[d, :, t0 // 64:t0 // 64 + nch, :])
            P.copy("pool", v_b[:, :nch, :], v_s[:, :nch, :])
            for c0 in range(0, tb, 512):
                cw = min(512, tb - c0)
                P.mm(pz[:, :cw], w2_sb[:, d, :], lr_s[:, c0:c0 + cw])
                P.act(e1[:, c0:c0 + cw], pz[:, :cw], AF.Exp, bias=nb2[:, d:d + 1], scale=-1.0)
            P.act(e1[:, :tb], e1[:, :tb], AF.Ln, bias=1.0)
            P.I("dve", lambda tb=tb: P.nc.vector.tensor_tensor_scan(out=cum.t[:, :tb], data0=m01_sb.t[:, :tb], data1=e1.t[:, :tb],
                                                             initial=0.0, op0=ALU.mult, op1=ALU.add), w=[cum], r=[m01_sb, e1])
            c3 = cum[:, :tb].rr("p (c t) -> p c t", t=64)
            d3 = d1[:, :tb].rr("p (c t) -> p c t", t=64)
            P.tt("dve", d3, c3, c3[:, :, 32:33].bc([64, nch, 64]), ALU.subtract)
            P.act(E[:, :tb], d1[:, :tb], AF.Exp, scale=1.0 / 16)
            P.tt("dve", ke[:, :tb], k_s[:, :tb], E[:, :tb], ALU.mult)
            P.act(E[:, :tb], d1[:, :tb], AF.Exp, scale=-1.0 / 16)
            P.stt(qe[:, :tb], q_s[:, :tb], 0.125, E[:, :tb], ALU.mult, ALU.mult)
            P.act(E[:, :tb], cum[:, :tb], AF.Exp, scale=-1.0 / 16)
            P.stt(qs[:, :tb], q_s[:, :tb], 0.125, E[:, :tb], ALU.mult, ALU.mult)
            P.act(dec[:, :nch], c3[:, :, 63], AF.Exp, scale=-1.0 / 16)
            P.tt("dve", d3, c3, c3[:, :, 63:64].bc([64, nch, 64]), ALU.subtract)
            P.act(E[:, :tb], d1[:, :tb], AF.Exp, scale=1.0 / 16)
            P.tt("dve", kdT[:, :tb], k_s[:, :tb], E[:, :tb], ALU.mult)
            for c8 in range(0, nch, 8):
                n8 = min(8, nch - c8)
                for j in range(n8):
                    P.tr(ptr[:, j * 64:(j + 1) * 64], kdT[:, (c8 + j) * 64:(c8 + j + 1) * 64], id_sb.all)
                P.copy("act", kd[:, c8:c8 + n8, :], ptr[:, :n8 * 64].rr("p (c t) -> p c t", t=64))
            for c in range(nch):
                b = c % 2
                cs = slice(c * 64, (c + 1) * 64)
                P.mm(pst[b].all, ke[:, cs], qe[:, cs])
                P.tt("dve", sT[b].all, pst[b].all, tri_sb.all, ALU.mult)
                P.mm(po[b].all, qs[:, cs], S_b.all, start=True, stop=False)
                P.mm(po[b].all, sT[b].all, v_b[:, c, :], start=False, stop=True)
                ob = o8[(c // 8) % 2]
                P.copy("act", ob[:, c % 8, :], po[b].all)
                P.mm(pds.all, kd[:, c, :], v_b[:, c, :])
                P.stt(S.all, S.all, dec[:, c:c + 1], pds.all, ALU.mult, ALU.add)
                P.copy("dve", S_b.all, S.all)
                if c % 8 == 7 or c == nch - 1:
                    n8 = c % 8 + 1
                    r0 = t0 + (c - n8 + 1) * 64
                    P.dma("pool", od[d][r0:r0 + n8 * 64, :].rr("(c p) f -> p c f", p=64), ob[:, :n8, :])
    nct = NCTX // 128; nlt = NLAT // 128
    f_s = [P.sb("fs", [128, 128]) for _ in range(2)]; b_s = [P.sb("bs", [128, 128]) for _ in range(2)]
    g_s = [P.sb("gs", [128, 128]) for _ in range(2)]; y_s = [P.sb("ys", [128, 128]) for _ in range(2)]
    yo = [P.sb("yo", [128, 128], BF16) for _ in range(2)]
    st = [P.sb("st", [128, 4]) for _ in range(2)]
    for i in range(nct + nlt):
        b = i % 2
        bi = (nct - 1 - i) if i < nct else nct + (nlt - 1 - (i - nct))
        P.dma("sp", f_s[b].all, od[0][i * 128:(i + 1) * 128, :])
        P.dma("pool", b_s[b].all, od[1][bi * 128:(bi + 1) * 128, :])
        P.dma("sp", g_s[b].all, g[i * 128:(i + 1) * 128, :])
        P.mm(pfin.all, J_sb.all, b_s[b].all)
        P.tt("dve", f_s[b].all, f_s[b].all, pfin.all, ALU.add)
        s = st[b]
        P.act(y_s[b].all, f_s[b].all, AF.Square, accum=s[:, 0:1])
        P.ts("dve", s[:, 1:2], s[:, 0:1], 1.0 / 128, EPS, op0=ALU.mult, op1=ALU.add)
        P.act(s[:, 1:2], s[:, 1:2], AF.Sqrt)
        P.recip(s[:, 2:3], s[:, 1:2])
        P.stt(y_s[b].all, f_s[b].all, s[:, 2:3], nw_sb.all, ALU.mult, ALU.mult)
        P.act(g_s[b].all, g_s[b].all, AF.Silu)
        P.tt("dve", yo[b].all, y_s[b].all, g_s[b].all, ALU.mult)
        P.dma("pool", o[i * 128:(i + 1) * 128, :], yo[b].all)
    return P.finish()


def build_gdn(NLAT, NCTX):
    P = Prog("gdn")
    P.force_sp = True
    import os
    P.skip_chunks = bool(os.environ.get("GDN_SKIP"))
    NT = NLAT + NCTX
    NTP = NT + 8
    SBK = 1024
    xT = P.din("xT", [2, 3, 128, NTP]); cw = P.din("cw", [2, 128, 3, 5])
    arow = P.din("arow", [2, 1, NT]); brow = P.din("brow", [2, 1, NT])
    sc = P.din("sc", [1, 4])
    acol = P.din("acol", [2, 64, NT // 64]); bcol = P.din("bcol", [2, 64, NT // 64])
    z = P.din("z", [NT, 128]); nw = P.din("nw", [128, 128])
    m01 = P.din("m01", [1, SBK]); msk = P.din("msk", [64, 4, 64])
    J = P.din("J", [128, 128]); idn = P.din("idn", [128, 128])
    o = P.dout("o", [NT, 128], BF16)
    od = [P.dtmp(f"od{d}", [NT, 128]) for d in range(2)]
    ld = lambda t, src: (P.dma("sp", t.all, src.all), t)[1]
    m01_sb = ld(P.sb("m01", [1, SBK]), m01); msk_sb = ld(P.sb("msk", [64, 4, 64]), msk)
    J_sb = ld(P.sb("J", [128, 128]), J); id_sb = ld(P.sb("idn", [128, 128]), idn); nw_sb = ld(P.sb("nw", [128, 128]), nw)
    sc_sb = ld(P.sb("sc", [1, 4]), sc)
    cw_sb = P.sb("cw", [128, 2, 3, 5]); P.dma("sp", cw_sb.all, cw.all.rr("d p a b -> p d a b"))
    ones = P.sb("ones", [128, 128]); P.memset("dve", ones.all, 1.0)
    negA = P.sb("negA", [1, 2]); P.act(negA.all, sc_sb[:, 0:2], AF.Exp); P.ts("dve", negA.all, negA.all, -1.0, None, op0=ALU.mult)
    sl, su, ui, I64 = (msk_sb[:, j, :] for j in range(4))
    xs = P.sb("xs", [128, SBK + 4]); acc = P.sb("acc", [128, SBK]); sqb = P.sb("sqb", [128, SBK]); rs = P.sb("rs", [128, SBK])
    qT = P.sb("qT", [128, SBK]); kT = P.sb("kT", [128, SBK]); qg = P.sb("qg", [128, SBK])
    v_tok = P.sb("vtok", [64, SBK // 64, 128]); k_tok = P.sb("ktok", [64, SBK // 64, 128])
    rows = {n: P.sb("r" + n, [1, SBK]) for n in ["a", "b", "g", "gc", "egc", "bg", "kdf", "t"]}
    gl = P.sb("gl", [1, SBK // 64]); egl = P.sb("egl", [1, SBK // 64])
    gc_bc = P.sb("gcbc", [64, SBK]); be_bc = P.sb("bebc", [64, SBK]); egc_bc = P.sb("egcbc", [128, SBK]); egl_bc = P.sb("eglbc", [128, SBK // 64])
    cA, cB, cGC, cBG, cKD = (P.sb("c" + n_, [64, SBK // 64]) for n_ in "abcde")
    sc64 = P.sb("sc64", [64, 4]); P.dma("sp", sc64.all, sc[0:1, :].bc([64, 4]))
    negA64 = P.sb("negA64", [64, 2]); P.act(negA64.all, sc64[:, 0:2], AF.Exp); P.ts("dve", negA64.all, negA64.all, -1.0, None, op0=ALU.mult)
    S = P.sb("S", [128, 128])
    tl = {n: P.sb(n, [64, 64]) for n in ["dM", "dN", "t1", "t2", "t3", "M0", "N0", "M1", "N1", "R", "at"]}
    vb = P.sb("vbb", [64, 128]); kbg = P.sb("kbg", [64, 128]); kd = P.sb("kd", [64, 128]); u_sb = P.sb("u", [64, 128])
    wT_sb = P.sb("wT", [128, 64]); vnew = P.sb("vnew", [64, 128])
    o8 = [P.sb("o8", [64, 8, 128]) for _ in range(2)]
    pbc = P.ps("pbc", [128, 512]); pcol = P.ps("pcol", [64, 512]); ptr = P.ps("ptr", [128, 512])
    pg = P.ps("pg", [64, 128]); plev = [P.ps("plev", [64, 128]) for _ in range(2)]
    p3 = P.ps("p3", [64, 384]); p4 = P.ps("p4", [128, 192])
    for d in range(2):
        P.memset("dve", S.all, 0.0)
        blocks = [(0, NCTX, 2)] + [(NCTX + b0, min(SBK, NLAT - b0), NCTX + 6 + b0) for b0 in range(0, NLAT, SBK)]
        for (t0, tb, p0) in blocks:
            nch = tb // 64
            for part, dst in ((0, qT), (1, kT), (2, None)):
                P.dma("sp", xs[:, :tb + 4], xT[d, part, :, p0 - 2:p0 + tb + 2])
                P.ts("dve", acc[:, :tb], xs[:, 0:tb], cw_sb[:, d, part, 0:1], None, op0=ALU.mult)
                for j in range(1, 5):
                    P.stt(acc[:, :tb], xs[:, j:j + tb], cw_sb[:, d, part, j:j + 1], acc[:, :tb], ALU.mult, ALU.add)
                P.act(acc[:, :tb], acc[:, :tb], AF.Silu)
                if dst is not None:
                    P.act(sqb[:, :tb], acc[:, :tb], AF.Square)
                    for c0 in range(0, tb, 512):
                        cwd = min(512, tb - c0)
                        P.mm(pbc[:, :cwd], ones.all, sqb[:, c0:c0 + cwd])
                        P.ts("dve", rs[:, c0:c0 + cwd], pbc[:, :cwd], EPS, None, op0=ALU.add)
                    P.act(rs[:, :tb], rs[:, :tb], AF.Sqrt)
                    P.recip(rs[:, :tb], rs[:, :tb])
                    if part == 0:
                        P.stt(dst[:, :tb], acc[:, :tb], 128 ** -0.5, rs[:, :tb], ALU.mult, ALU.mult)
                    else:
                        P.tt("dve", dst[:, :tb], acc[:, :tb], rs[:, :tb], ALU.mult)
                src = acc if dst is None else (kT if part == 1 else None)
                tok = v_tok if dst is None else (k_tok if part == 1 else None)
                if src is not None:
                    for c4 in range(0, nch, 4):
                        n4 = min(4, nch - c4)
                        for j in range(n4):
                            P.tr(ptr[0:64, j * 128:(j + 1) * 128], src[:, (c4 + j) * 64:(c4 + j + 1) * 64], id_sb.all)
                        P.copy("act", tok[:, c4:c4 + n4, :], ptr[0:64, :n4 * 128].rr("p (c f) -> p c f", f=128))
            R_ = rows
            P.dma("sp", R_["a"][:, :tb], arow[d, :, t0:t0 + tb])
            P.dma("sp", R_["b"][:, :tb], brow[d, :, t0:t0 + tb])
            P.act(R_["b"][:, :tb], R_["b"][:, :tb], AF.Sigmoid)
            P.act(R_["t"][:, :tb], R_["a"][:, :tb], AF.Exp, bias=sc_sb[:, 2 + d:3 + d], scale=1.0)
            P.act(R_["t"][:, :tb], R_["t"][:, :tb], AF.Ln, bias=1.0)
            P.ts("dve", R_["g"][:, :tb], R_["t"][:, :tb], negA[:, d:d + 1], None, op0=ALU.mult)
            P.I("dve", lambda tb=tb: P.nc.vector.tensor_tensor_scan(out=R_["gc"].t[:, :tb], data0=m01_sb.t[:, :tb], data1=R_["g"].t[:, :tb],
                                                             initial=0.0, op0=ALU.mult, op1=ALU.add), w=[R_["gc"]], r=[m01_sb, R_["g"]])
            P.act(R_["egc"][:, :tb], R_["gc"][:, :tb], AF.Exp)
            P.tt("dve", R_["bg"][:, :tb], R_["b"][:, :tb], R_["egc"][:, :tb], ALU.mult)
            g3 = R_["gc"][:, :tb].rr("p (c t) -> p c t", t=64)
            P.copy("dve", gl[:, :nch], g3[:, :, 63])
            P.act(egl[:, :nch], gl[:, :nch], AF.Exp)
            P.tt("dve", R_["kdf"][:, :tb].rr("p (c t) -> p c t", t=64), gl[:, :nch].rr("p (c o) -> p c o", o=1).bc([1, nch, 64]), g3, ALU.subtract)
            P.act(R_["kdf"][:, :tb], R_["kdf"][:, :tb], AF.Exp)
            for c0 in range(0, tb, 512):
                cwd = min(512, tb - c0)
                for nm, dstb, npart in (("gc", gc_bc, 64), ("b", be_bc, 64), ("egc", egc_bc, 128)):
                    P.mm(pbc[:npart, :cwd], ones[0:1, 0:npart], R_[nm][:, c0:c0 + cwd])
                    P.copy("act", dstb[:, c0:c0 + cwd], pbc[:npart, :cwd])
            P.mm(pbc[:, :nch], ones[0:1, 0:128], egl[:, :nch])
            P.copy("act", egl_bc[:, :nch], pbc[:, :nch])
            c0i = t0 // 64
            P.dma("sp", cA[:, :nch], acol[d, :, c0i:c0i + nch])
            P.dma("sp", cB[:, :nch], bcol[d, :, c0i:c0i + nch])
            P.act(cB[:, :nch], cB[:, :nch], AF.Sigmoid)
            P.act(cA[:, :nch], cA[:, :nch], AF.Exp, bias=sc64[:, 2 + d:3 + d], scale=1.0)
            P.act(cA[:, :nch], cA[:, :nch], AF.Ln, bias=1.0)
            P.ts("dve", cA[:, :nch], cA[:, :nch], negA64[:, d:d + 1], None, op0=ALU.mult)
            P.mm(pcol[:, 0:nch], ui, cA[:, :nch])
            P.mm(pcol[:, 64:64 + nch], ones[0:64, 0:64], cA[:, :nch])
            P.copy("dve", cGC[:, :nch], pcol[:, 0:nch])
            P.tt("dve", cKD[:, :nch], pcol[:, 64:64 + nch], cGC[:, :nch], ALU.subtract)
            P.act(cKD[:, :nch], cKD[:, :nch], AF.Exp)
            P.act(cBG[:, :nch], cGC[:, :nch], AF.Exp)
            P.tt("dve", cBG[:, :nch], cBG[:, :nch], cB[:, :nch], ALU.mult)
            P.tt("dve", qg[:, :tb], qT[:, :tb], egc_bc[:, :tb], ALU.mult)
            for c in range(nch if not getattr(P, "skip_chunks", False) else 0):
                cs = slice(c * 64, (c + 1) * 64)
                gcc, bec, bgc, kdc = (t_[:, c:c + 1] for t_ in (cGC, cB, cBG, cKD))
                P.mm(pg[:, 0:64], kT[:, cs], kT[:, cs])
                P.mm(pg[:, 64:128], kT[:, cs], qT[:, cs])
                P.ts("dve", tl["dM"].all, gc_bc[:, cs], gcc, 0.0, op0=ALU.subtract, op1=ALU.max)
                P.act(tl["dM"].all, tl["dM"].all, AF.Exp, scale=-1.0)
                P.ts("dve", tl["dN"].all, gc_bc[:, cs], gcc, 0.0, op0=ALU.subtract, op1=ALU.min)
                P.act(tl["dN"].all, tl["dN"].all, AF.Exp)
                P.stt(tl["t1"].all, pg[:, 0:64], bec, tl["dM"].all, ALU.mult, ALU.mult)
                P.tt("dve", tl["M0"].all, tl["t1"].all, sl, ALU.mult)
                P.tt("dve", tl["t2"].all, pg[:, 0:64], be_bc[:, cs], ALU.mult)
                P.tt("dve", tl["t2"].all, tl["t2"].all, tl["dN"].all, ALU.mult)
                P.tt("dve", tl["N0"].all, tl["t2"].all, su, ALU.mult)
                P.tt("dve", tl["t3"].all, pg[:, 64:128], tl["dN"].all, ALU.mult)
                P.tt("dve", tl["at"].all, tl["t3"].all, ui, ALU.mult)
                P.tt("dve", tl["R"].all, I64, tl["N0"].all, ALU.subtract)
                Mc, Nc, Mn, Nn = tl["M0"], tl["N0"], tl["M1"], tl["N1"]
                stage = int(os.environ.get("GDN_STAGE", "9"))
                if stage <= 1:
                    continue
                for lev in range(1, 6):
                    pl = plev[lev % 2]
                    P.mm(pl[:, 0:64], Nc.all, Mc.all)
                    if lev < 5:
                        P.mm(pl[:, 64:128], Mc.all, Nc.all)
                        P.copy("dve", Nn.all, pl[:, 64:128])
                    P.copy("dve", Mn.all, pl[:, 0:64])
                    pr = plev[(lev + 1) % 2]
                    P.mm(pr[:, 0:64], Mn.all, tl["R"].all)
                    P.tt("dve", tl["R"].all, tl["R"].all, pr[:, 0:64], ALU.add)
                    Mc, Nc, Mn, Nn = Mn, Nn, Mc, Nc
                if stage <= 2:
                    continue
                P.ts("dve", vb.all, v_tok[:, c, :], bec, None, op0=ALU.mult)
                P.ts("dve", kbg.all, k_tok[:, c, :], bgc, None, op0=ALU.mult)
                P.ts("dve", kd.all, k_tok[:, c, :], kdc, None, op0=ALU.mult)
                P.mm(p3[:, 0:128], tl["R"].all, vb.all)
                P.mm(p4[:, 0:64], kbg.all, tl["R"].all)
                P.copy("act", u_sb.all, p3[:, 0:128])
                P.copy("dve", wT_sb.all, p4[:, 0:64])
                P.mm(p3[:, 128:256], wT_sb.all, S.all)
                P.tt("dve", vnew.all, u_sb.all, p3[:, 128:256], ALU.subtract)
                P.mm(p3[:, 256:384], qg[:, cs], S.all, start=True, stop=False)
                P.mm(p3[:, 256:384], tl["at"].all, vnew.all, start=False, stop=True)
                ob = o8[(c // 8) % 2]
                P.copy("act", ob[:, c % 8, :], p3[:, 256:384])
                P.mm(p4[:, 64:192], kd.all, vnew.all)
                P.stt(S.all, S.all, egl_bc[:, c:c + 1], p4[:, 64:192], ALU.mult, ALU.add)
                if c % 8 == 7 or c == nch - 1:
                    n8 = c % 8 + 1
                    r0 = t0 + (c - n8 + 1) * 64
                    P.dma("pool", od[d][r0:r0 + n8 * 64, :].rr("(c p) f -> p c f", p=64), ob[:, :n8, :])
    nct = NCTX // 128; nlt = NLAT // 128
    f_s = [P.sb("fs", [128, 128]) for _ in range(2)]; b_s = [P.sb("bs", [128, 128]) for _ in range(2)]
    g_s = [P.sb("gs", [128, 128]) for _ in range(2)]; y_s = [P.sb("ys", [128, 128]) for _ in range(2)]
    yo = [P.sb("yo", [128, 128], BF16) for _ in range(2)]
    st = [P.sb("st", [128, 4]) for _ in range(2)]
    for i in range(nct + nlt):
        b = i % 2
        bi = (nct - 1 - i) if i < nct else nct + (nlt - 1 - (i - nct))
        P.dma("sp", f_s[b].all, od[0][i * 128:(i + 1) * 128, :])
        P.dma("pool", b_s[b].all, od[1][bi * 128:(bi + 1) * 128, :])
        P.dma("sp", g_s[b].all, z[i * 128:(i + 1) * 128, :])
        P.mm(ptr[:, 0:128], J_sb.all, b_s[b].all)
        P.tt("dve", f_s[b].all, f_s[b].all, ptr[:, 0:128], ALU.add)
        s = st[b]
        P.act(y_s[b].all, f_s[b].all, AF.Square, accum=s[:, 0:1])
        P.ts("dve", s[:, 1:2], s[:, 0:1], 1.0 / 128, EPS, op0=ALU.mult, op1=ALU.add)
        P.act(s[:, 1:2], s[:, 1:2], AF.Sqrt)
        P.recip(s[:, 2:3], s[:, 1:2])
        P.stt(y_s[b].all, f_s[b].all, s[:, 2:3], nw_sb.all, ALU.mult, ALU.mult)
        P.act(g_s[b].all, g_s[b].all, AF.Silu)
        P.tt("dve", yo[b].all, y_s[b].all, g_s[b].all, ALU.mult)
        P.dma("pool", o[i * 128:(i + 1) * 128, :], yo[b].all)
    return P.finish()


def gdn_masks():
    i = np.arange(64)[:, None]; j = np.arange(64)[None, :]
    sl = (i > j); su = (j > i); uiq = (j >= i); I = (i == j)
    return np.ascontiguousarray(np.stack([sl, su, uiq, I], 1).astype(np.float32))


def build_merge(NTOK):
    P = Prog("merge")
    oT = P.din("oT", [128, 32, NTOK], BF16)
    G = P.din("G", [NTOK, 2048], BF16)
    Wb = P.din("Wb", [32, 128, 512])
    z = P.dout("z", [NTOK, 512], BF16)
    wb = P.sb("wb", [128, 32, 512], BF16)
    stg = [P.sb("stg", [128, 512]) for _ in range(2)]
    for kc in range(32):
        P.dma("sp" if kc % 2 else "pool", stg[kc % 2].all, Wb[kc])
        P.copy("dve" if kc % 2 else "pool", wb[:, kc, :], stg[kc % 2].all)
    ab = [P.sb("ab", [128, 32, 512], BF16) for _ in range(2)]
    gt = [P.sb("gt", [128, 2048], BF16) for _ in range(2)]
    acc = [P.sb("acc", [128, 512]) for _ in range(2)]
    tmp = [P.sb("tmp", [128, 512]) for _ in range(2)]
    zt = [P.sb("zt", [128, 512], BF16) for _ in range(2)]
    ps = [P.ps("ps", [128, 512]) for _ in range(8)]
    it = 0
    for bi, t0 in enumerate(range(0, NTOK, 512)):
        tb = min(512, NTOK - t0)
        a = ab[bi % 2]
        P.dma("sp", a[:, :, :tb], oT[:, :, t0:t0 + tb])
        for tt_ in range(0, tb, 128):
            g = gt[it % 2]
            P.dma("pool", g.all, G[t0 + tt_:t0 + tt_ + 128, :])
            for i in range(4):
                p = ps[(it % 2) * 4 + i]
                for kc in range(8):
                    P.mm(p.all, a[:, i * 8 + kc, tt_:tt_ + 128], wb[:, i * 8 + kc, :], start=(kc == 0), stop=(kc == 7))
            ac = acc[it % 2]
            P.tt("dve", ac.all, ps[(it % 2) * 4].all, g[:, 0:512], ALU.mult)
            for i in range(1, 4):
                tm = tmp[i % 2]
                P.tt("dve", tm.all, ps[(it % 2) * 4 + i].all, g[:, i * 512:(i + 1) * 512], ALU.mult)
                if i < 3:
                    P.tt("pool", ac.all, ac.all, tm.all, ALU.add)
                else:
                    P.tt("pool", zt[it % 2].all, ac.all, tm.all, ALU.add)
            P.dma("sp", z[t0 + tt_:t0 + tt_ + 128, :], zt[it % 2].all)
            it += 1
    return P.finish()


def build_moe(NLAT, NCTX):
    P = Prog("moe")
    NTOK = NLAT + NCTX
    NTL = NTOK // 128
    F = 1024
    cap_l = 2 * NLAT // 16
    cap_c = 2 * NCTX // 16
    xT = P.din("xT", [128, 32, NTOK], BF16)
    affc = P.din("affc", [128, NTL, 2])
    W1 = P.din("W1", [2, D, F]); W3 = P.din("W3", [2, D, F]); W2 = P.din("W2", [2, F, D])
    Y = P.dout("Y", [NTOK, D], BF16)
    Hs = [P.dtmp(f"Hs{e}", [128, 8, NTOK], BF16) for e in range(2)]
    ac = P.sb("ac", [128, NTL, 2]); P.dma("sp", ac.all, affc.all)
    Gt = P.sb("Gt", [128, NTL, 2])
    nll = NLAT // 128
    ones = P.sb("ones", [128, 128]); P.memset("dve", ones.all, 1.0)
    lo = P.sb("lo", [128, 4]); P.memset("dve", lo.all, 0.0)
    mid = P.sb("mid", [128, 4]); cnt = P.sb("cnt", [128, 4]); ge = P.sb("ge", [128, 4])
    junk = P.sb("junk", [128, NTL])
    pthr = P.ps("pthr", [128, 4])
    views = [(ac[:, :nll, 0], nll), (ac[:, :nll, 1], nll), (ac[:, nll:, 0], NTL - nll), (ac[:, nll:, 1], NTL - nll)]
    for k in range(40):
        wk_ = 0.5 ** (k + 1)
        P.ts("dve", mid.all, lo.all, wk_, None, op0=ALU.add)
        for j, (v_, n_) in enumerate(views):
            P.ts("dve", junk[:, :n_], v_, mid[:, j:j + 1], 0.0, op0=ALU.is_ge, op1=ALU.add, accum=cnt[:, j:j + 1])
        P.mm(pthr.all, ones.all, cnt.all)
        P.ts("dve", ge[:, 0:2], pthr[:, 0:2], cap_l - 0.5, None, op0=ALU.is_ge)
        P.ts("dve", ge[:, 2:4], pthr[:, 2:4], cap_c - 0.5, None, op0=ALU.is_ge)
        P.stt(lo.all, ge.all, wk_, lo.all, ALU.mult, ALU.add)
    for e in range(2):
        P.ts("dve", Gt[:, :nll, e], ac[:, :nll, e], lo[:, e:e + 1], None, op0=ALU.is_ge)
        P.ts("dve", Gt[:, nll:, e], ac[:, nll:, e], lo[:, 2 + e:3 + e], None, op0=ALU.is_ge)
    P.tt("dve", Gt.all, Gt.all, ac.all, ALU.mult)
    Wbig = P.sb("Wbig", [128, 2 * 32 * F], BF16)
    Wv = Wbig.all.rr("p (w k f) -> p w k f", w=2, k=32)
    stg = [P.sb("stg", [128, F]) for _ in range(2)]
    TB = 256
    xb = [P.sb("xb", [128, 32, TB], BF16) for _ in range(2)]
    hb = [P.sb("hb", [128, 8, TB], BF16) for _ in range(2)]
    h1 = [P.sb("h1", [128, TB]) for _ in range(2)]
    pf = [P.ps("pf", [128, 512]) for _ in range(6)]
    k = 0
    for e in range(2):
        for wi, Wsrc in enumerate((W1, W3)):
            for kc in range(32):
                s = stg[k % 2]
                P.dma("sp" if k % 2 else "pool", s.all, Wsrc[e, kc * 128:(kc + 1) * 128, :])
                P.copy("pool", Wv[:, wi, kc, :], s.all)
                k += 1
        pi = 0
        for bi, t0 in enumerate(range(0, NTOK, TB)):
            tb = min(TB, NTOK - t0)
            x = xb[bi % 2]
            P.dma("sp", x[:, :, :tb], xT[:, :, t0:t0 + tb])
            hh = hb[bi % 2]
            for fc in range(8):
                p1 = pf[pi % 6]; p3 = pf[(pi + 1) % 6]; pi += 2
                for kc in range(32):
                    P.mm(p1[:, :tb], Wv[:, 0, kc, fc * 128:(fc + 1) * 128], x[:, kc, :tb], start=(kc == 0), stop=(kc == 31))
                for kc in range(32):
                    P.mm(p3[:, :tb], Wv[:, 1, kc, fc * 128:(fc + 1) * 128], x[:, kc, :tb], start=(kc == 0), stop=(kc == 31))
                hs = h1[fc % 2]
                P.act(hs[:, :tb], p1[:, :tb], AF.Silu)
                P.tt("dve", hh[:, fc, :tb], hs[:, :tb], p3[:, :tb], ALU.mult)
            P.dma("pool", Hs[e][:, :, t0:t0 + tb], hh[:, :, :tb])
    W2v = Wbig.all.rr("p (e f n) -> p e f n", e=2, f=8)
    for e in range(2):
        for fc in range(8):
            for half in range(4):
                s = stg[k % 2]
                P.dma("sp" if k % 2 else "pool", s.all, W2[e, fc * 128:(fc + 1) * 128, half * 1024:(half + 1) * 1024])
                P.copy("pool", W2v[:, e, fc, half * 1024:(half + 1) * 1024], s.all)
                k += 1
    ht = [[P.sb("ht", [128, 8, 128], BF16) for _ in range(2)] for _ in range(2)]
    ysb = [P.sb("ysb", [128, 512]) for _ in range(2)]
    ysh = [P.sb("ysh", [128, 2048], BF16) for _ in range(2)]
    pi = 0
    for t in range(NTL):
        for e in range(2):
            P.dma("sp" if e else "pool", ht[e][t % 2].all, Hs[e][:, :, t * 128:(t + 1) * 128])
        for n in range(8):
            yb = ysb[n % 2]
            nn = n % 4
            p0 = pf[pi % 6]; p1 = pf[(pi + 1) % 6]; pi += 2
            for e, p in ((0, p0), (1, p1)):
                for fc in range(8):
                    P.mm(p.all, ht[e][t % 2][:, fc, :], W2v[:, e, fc, n * 512:(n + 1) * 512], start=(fc == 0), stop=(fc == 7))
            P.act(yb.all, p0.all, AF.Copy, scale=Gt[:, t, 0:1])
            yh = ysh[(n // 4) % 2]
            P.stt(yh[:, nn * 512:(nn + 1) * 512], p1.all, Gt[:, t, 1:2], yb.all, ALU.mult, ALU.add)
            if nn == 3:
                hf = n // 4
                P.dma("sp" if hf else "pool", Y[t * 128:(t + 1) * 128, hf * 2048:(hf + 1) * 2048], yh.all)
    return P.finish()


NCTX_ = 256
_PROGS = {}


def _prog(key, fn):
    if key not in _PROGS:
        _PROGS[key] = fn()
    return _PROGS[key]


def _pack_tok(lat, ctx, nlt):
    out = []
    per = nlt * 128
    for c in range(NCORE):
        a = np.zeros((nlt + 1, 128, lat.shape[1]), lat.dtype)
        a[:nlt] = lat[c * per:(c + 1) * per].reshape(nlt, 128, -1)
        a[nlt, :32] = ctx[c * 32:(c + 1) * 32]
        out.append(a)
    return out


def _unpack_tok(arrs, nlt):
    lat = np.concatenate([a[:nlt].reshape(nlt * 128, -1) for a in arrs], 0)
    ctx = np.concatenate([a[nlt, :32] for a in arrs], 0)
    return lat, ctx


def _fm(a, kc):
    n = a.shape[0]
    return np.ascontiguousarray(a.reshape(n, kc, 128).transpose(2, 1, 0))


OFF = {}
_o = 0
for _n, _w in (('na_q', 1024), ('na_k', 1024), ('na_v', 1024), ('gla_q', 512), ('gla_k', 512), ('gla_v', 1024), ('gla_g', 1024),
               ('gla_lr', 32), ('mla_cq', 1024), ('mla_ckv', 512), ('mla_kr', 64), ('gdn_qkv', 3072), ('gdn_z', 1024),
               ('gdn_b', 16), ('gdn_a', 16)):
    OFF[_n] = (_o, _o + _w)
    _o += _w
NG = _o


def _rowops_vec(g, sc, sh, nw):
    return np.ascontiguousarray(np.stack([np.stack([g[k], sc[k], sh[k], nw]) for k in range(2)]).astype(np.float32))


def kernel_impl(inp, NLAT):
    NCTX = NCTX_
    NTOK = NLAT + NCTX
    ROWS = NLAT // 64
    nlt = NLAT // 128 // NCORE
    NT9 = nlt + 1
    x = np.asarray(inp["x"])[0][:NLAT]
    ctx = np.asarray(inp["ctx"])[0]
    g = lambda k: np.asarray(inp[k])
    mod = run_adaln(g("c"), g("c_ctx"), g("ada_w"), g("ada_b"))
    ident128 = np.eye(128, dtype=np.float32)
    J128 = np.ascontiguousarray(ident128[::-1])
    zeros = np.zeros(D, np.float32)
    cos, sins = rope_tables(NLAT)
    h_pk = _pack_tok(x, ctx, nlt)
    u_pk = None
    for l in range(2):
        sh1, sc1, g1, sh2, sc2, g2 = [[mod[l, m, i * D:(i + 1) * D] for m in range(2)] for i in range(6)]
        if l == 0:
            nc = _prog(("r0", NT9), lambda: build_rowops(NT9, nlt, 0, False, want_h=False))
            vec = _rowops_vec([zeros, zeros], sc1, sh1, g("norm1_w")[0])
            res = run(nc, [{"h": h_pk[c], "vec": vec} for c in range(NCORE)])
            u_pk = [r["u"] for r in res]
        u_lat, u_ctx = _unpack_tok(u_pk, nlt)
        uT = _fm(np.concatenate([u_lat, u_ctx], 0), 32)
        w_in = g("w_in")[l]
        nb = [(NG // NCORE, "f32"), (1024, "sig_bf16"), (1024, "sig_bf16")]
        nc = _prog(("gin", NTOK), lambda: build_gemm(32, NTOK, nb))
        maps = []
        npc = NG // NCORE
        for c in range(NCORE):
            cols = [w_in[:, c * npc:(c + 1) * npc]] + [w_in[:, NG + i * D + c * 512:NG + i * D + (c + 1) * 512] for i in range(4)]
            maps.append({"AT": uT, "W": np.ascontiguousarray(np.concatenate(cols, 1))})
        res = run(nc, maps)
        del maps
        Pn = np.concatenate([r["O0"] for r in res], 1)
        Gs = [np.concatenate([r["O1"], r["O2"]], 1) for r in res]
        del res
        Pl, Pc = Pn[:NLAT], Pn[NLAT:]
        col = lambda name, a, b: (Pl[:, OFF[name][0] + a:OFF[name][0] + b], Pc[:, OFF[name][0] + a:OFF[name][0] + b])
        o_all = np.zeros((NTOK, 4, 8, 128), Gs[0].dtype)
        nc = _prog(("na", ROWS), lambda: build_na(ROWS, NCTX))
        maps = []
        id64 = np.eye(64, dtype=np.float32)
        for hh in range(8):
            cat = lambda name: np.concatenate(col(name, hh * 128, (hh + 1) * 128), 0)
            qa, ka, va = cat('na_q'), cat('na_k'), cat('na_v')
            maps.append({"qT": np.ascontiguousarray(qa.T), "kT": np.ascontiguousarray(ka.T),
                         "v": np.ascontiguousarray(va.reshape(-1, 64, 128).transpose(1, 0, 2)),
                         "bias": na_bias_table(g("na_rpb")[l, hh]), "ident": id64})
        res = run(nc, maps)
        for hh in range(8):
            o_all[:, 0, hh] = res[hh]["o"]
        nc = _prog(("mla", NLAT), lambda: build_mla(NLAT, NCTX))
        cqa = np.concatenate(col('mla_cq', 0, 1024), 0); ckva = np.concatenate(col('mla_ckv', 0, 512), 0)
        kra = np.concatenate(col('mla_kr', 0, 64), 0)
        cqT, ckvT = _fm(cqa, 8), _fm(ckva, 4)
        krT = np.ascontiguousarray(kra.T); krsT = np.ascontiguousarray(kra[:, SWAP64].T)
        nwq = np.ascontiguousarray(g("mla_q_norm_w")[l].reshape(8, 128).T); nwkv = np.ascontiguousarray(g("mla_kv_norm_w")[l].reshape(4, 128).T)
        maps = []
        for hh in range(8):
            wq = g("mla_w_uq")[l][:, hh * 192:(hh + 1) * 192]; wkv = g("mla_w_ukv")[l][:, hh * 256:(hh + 1) * 256]
            maps.append({"cqT": cqT, "ckvT": ckvT, "krT": krT, "krsT": krsT, "nwq": nwq, "nwkv": nwkv,
                         "wuq": np.ascontiguousarray(wq.reshape(8, 128, 192).transpose(1, 0, 2)),
                         "wuqs": np.ascontiguousarray(wq[:, 128:][:, SWAP64].reshape(8, 128, 64).transpose(1, 0, 2)),
                         "wukv": np.ascontiguousarray(wkv.reshape(4, 128, 256).transpose(1, 0, 2)),
                         "cos": cos, "sins": sins, "ident": ident128})
        res = run(nc, maps)
        for hh in range(8):
            o_all[:, 2, hh] = res[hh]["o"]
        del cqT, ckvT, maps

        def seq(name, a, b, d):
            lt, ct = col(name, a, b)
            if d == 1:
                lt = lt[::-1]; ct = ct[::-1]
            return np.concatenate([ct, lt], 0)
        nc = _prog(("gla", NLAT), lambda: build_gla(NLAT, NCTX))
        m01 = np.ones((64, 2048), np.float32); m01[:, ::64] = 0
        tri = np.triu(np.ones((64, 64), np.float32))
        maps = []
        for hh in range(8):
            qT = np.stack([np.ascontiguousarray(seq('gla_q', hh * 64, (hh + 1) * 64, d).T) for d in range(2)])
            kT = np.stack([np.ascontiguousarray(seq('gla_k', hh * 64, (hh + 1) * 64, d).T) for d in range(2)])
            vv = np.stack([np.ascontiguousarray(seq('gla_v', hh * 128, (hh + 1) * 128, d).reshape(-1, 64, 128).transpose(1, 0, 2)) for d in range(2)])
            lrT = np.stack([np.ascontiguousarray(seq('gla_lr', d * 16, (d + 1) * 16, d).T) for d in range(2)])
            maps.append({"qT": qT, "kT": kT, "v": vv, "lrT": lrT,
                         "w2": np.ascontiguousarray(g("gla_w2")[l][:, :, hh * 64:(hh + 1) * 64]),
                         "b2": np.ascontiguousarray(g("gla_b2")[l][:, hh * 64:(hh + 1) * 64].T),
                         "g": np.ascontiguousarray(seq('gla_g', hh * 128, (hh + 1) * 128, 0)),
                         "nw": np.ascontiguousarray(np.broadcast_to(g("gla_norm_w")[l][hh * 128:(hh + 1) * 128], (128, 128))),
                         "m01": m01, "tri": tri, "id64": id64, "J": J128})
        res = run(nc, maps)
        for hh in range(8):
            ob = res[hh]["o"]
            o_all[:NLAT, 1, hh] = ob[NCTX:]; o_all[NLAT:, 1, hh] = ob[:NCTX]
        nc = _prog(("gdn", NLAT), lambda: build_gdn(NLAT, NCTX))
        m01r = np.ones((1, 1024), np.float32); m01r[:, ::64] = 0
        msk = gdn_masks()
        conv_w = g("gdn_conv_w")[l]
        maps = []
        for hh in range(8):
            xT = np.zeros((2, 3, 128, NTOK + 8), np.float32); cwm = np.zeros((2, 128, 3, 5), np.float32)
            arow = np.zeros((2, 1, NTOK), np.float32); brow = np.zeros((2, 1, NTOK), np.float32)
            for d in range(2):
                for part in range(3):
                    lt, ct = col('gdn_qkv', part * 1024 + hh * 128, part * 1024 + (hh + 1) * 128)
                    w = conv_w[:, part * 1024 + hh * 128:part * 1024 + (hh + 1) * 128]
                    if d == 1:
                        lt = lt[::-1]; ct = ct[::-1]; w = w[::-1]
                    xT[d, part, :, 2:2 + NCTX] = ct.T; xT[d, part, :, NCTX + 6:NCTX + 6 + NLAT] = lt.T
                    cwm[d, :, part, :] = w.T
                arow[d, 0] = seq('gdn_a', d * 8 + hh, d * 8 + hh + 1, d)[:, 0]
                brow[d, 0] = seq('gdn_b', d * 8 + hh, d * 8 + hh + 1, d)[:, 0]
            maps.append({"xT": xT, "cw": cwm, "arow": arow, "brow": brow,
                         "acol": np.ascontiguousarray(arow.reshape(2, -1, 64).transpose(0, 2, 1)),
                         "bcol": np.ascontiguousarray(brow.reshape(2, -1, 64).transpose(0, 2, 1)),
                         "sc": np.array([[g("gdn_a_log")[l, 0, hh], g("gdn_a_log")[l, 1, hh],
                                          g("gdn_dt_bias")[l, 0, hh], g("gdn_dt_bias")[l, 1, hh]]], np.float32),
                         "z": np.ascontiguousarray(seq('gdn_z', hh * 128, (hh + 1) * 128, 0)),
                         "nw": np.ascontiguousarray(np.broadcast_to(g("gdn_norm_w")[l], (128, 128))),
                         "m01": m01r, "msk": msk, "J": J128, "idn": ident128})
        res = run(nc, maps)
        for hh in range(8):
            ob = res[hh]["o"]
            o_all[:NLAT, 3, hh] = ob[NCTX:]; o_all[NLAT:, 3, hh] = ob[:NCTX]
        del maps, Pn, Pl, Pc
        oT = np.ascontiguousarray(o_all.transpose(3, 1, 2, 0).reshape(128, 32, NTOK))
        nc = _prog(("merge", NTOK), lambda: build_merge(NTOK))
        w_br = g("w_br")[l]
        res = run(nc, [{"oT": oT, "G": Gs[c],
                        "Wb": np.ascontiguousarray(w_br[:, :, c * 512:(c + 1) * 512].reshape(32, 128, 512))} for c in range(NCORE)])
        z = np.concatenate([r["z"] for r in res], 1)
        del Gs, oT
        zT = _fm(z, 32)
        nc = _prog(("go", NTOK), lambda: build_gemm(32, NTOK, [(512, "f32")]))
        w_o = g("w_o")[l]
        res = run(nc, [{"AT": zT, "W": np.ascontiguousarray(w_o[:, c * 512:(c + 1) * 512])} for c in range(NCORE)])
        y = np.concatenate([r["O0"] for r in res], 1)
        nc = _prog(("r1", NT9), lambda: build_rowops(NT9, nlt, 1, True))
        vec = _rowops_vec(g1, sc2, sh2, g("norm2_w")[l])
        y_pk = _pack_tok(y[:NLAT], y[NLAT:], nlt)
        rw = np.ascontiguousarray(g("router_w")[l].reshape(32, 128, 16).transpose(1, 0, 2))
        res = run(nc, [{"h": h_pk[c], "y0": y_pk[c], "vec": vec, "rw": rw, "ident": ident128} for c in range(NCORE)])
        h_pk = [r["hn"] for r in res]
        u2_lat, u2_ctx = _unpack_tok([r["u"] for r in res], nlt)
        a_lat, a_ctx = _unpack_tok([r["aff"] for r in res], nlt)
        aff = np.concatenate([a_lat, a_ctx], 0)
        u2T = _fm(np.concatenate([u2_lat, u2_ctx], 0), 32)
        del y, y_pk, z, zT
        nc = _prog(("moe", NLAT), lambda: build_moe(NLAT, NCTX))
        maps = []
        for c in range(NCORE):
            es = slice(2 * c, 2 * c + 2)
            maps.append({"xT": u2T, "affc": np.ascontiguousarray(aff[:, es].reshape(NTOK // 128, 128, 2).transpose(1, 0, 2)),
                         "W1": g("exp_w1")[l][es], "W3": g("exp_w3")[l][es], "W2": g("exp_w2")[l][es]})
        res = run(nc, maps)
        del maps, u2T
        parts = [_pack_tok(r["Y"][:NLAT], r["Y"][NLAT:], nlt) for r in res]
        del res
        last = (l == 1)
        if not last:
            nc = _prog(("r8", NT9), lambda: build_rowops(NT9, nlt, 8, False, pdt=BF16))
            sh1n, sc1n = [[mod[l + 1, m, i * D:(i + 1) * D] for m in range(2)] for i in range(2)]
            vec = _rowops_vec(g2, sc1n, sh1n, g("norm1_w")[l + 1])
        else:
            nc = _prog(("r8f", NT9), lambda: build_rowops(NT9, nlt, 8, False, want_h=False, udt=F32, pdt=BF16))
            vec = _rowops_vec(g2, [zeros, zeros], [zeros, zeros], g("final_norm_w"))
        maps = []
        for c in range(NCORE):
            m = {"h": h_pk[c], "vec": vec}
            for i in range(8):
                m[f"y{i}"] = parts[i][c]
            maps.append(m)
        res = run(nc, maps)
        del maps, parts
        if not last:
            h_pk = [r["hn"] for r in res]
            u_pk = [r["u"] for r in res]
        else:
            out_lat, _ = _unpack_tok([r["u"] for r in res], nlt)
            return out_lat[None].astype(np.float32)


def kernel(**inputs):
    return kernel_impl(inputs, 8192)
```
